# Optimizing a Trainium2 kernel written in Bass

```python
import math
import jax, jax.numpy as jnp
from jax import lax
import numpy as np

D_MODEL = 1024
BATCH = 4
SEQ = 8192
DEPTH = 4

CHUNK = 64
N_MIXERS = 2
N_POOL = (DEPTH + 1) // 2
N_FOX = DEPTH // 2
POOL_WINDOWS = (2, 4, 8, 16)
N_POOL_GROUPS = len(POOL_WINDOWS)
POOL_GROUP = D_MODEL // N_POOL_GROUPS
HEAD_DIM = 64
N_HEADS = D_MODEL // HEAD_DIM
Q_BLOCK = 128
NEG_INF = -1e30
N_EXPERTS = 32
TOP_K = 4
D_EXPERT = D_MODEL
SWIGLU_LIMIT = 7.0
SWIGLU_ALPHA = 1.702
EXPERT_BLOCK = 128
EPS = 1e-6

kernel_name = 'hybrid_pool_fox_moe_adaln_encoder'


def rmsnorm(x, g):
    x32 = x.astype(jnp.float32)
    y = x32 * lax.rsqrt(jnp.mean(x32 * x32, axis=-1, keepdims=True) + EPS)
    return (y * g.astype(jnp.float32)).astype(x.dtype)


def modulate(h, shift, scale):
    return h * (1 + scale[:, None, :]) + shift[:, None, :]


def pool_mixer(h, w_grp, scale):
    B, S, D = h.shape
    h32 = h.astype(jnp.float32)
    cs = jnp.concatenate([jnp.zeros((B, 1, D), jnp.float32), jnp.cumsum(h32, axis=1)], axis=1)
    t = jnp.arange(S)
    parts = []
    for g, w in enumerate(POOL_WINDOWS):
        sl = slice(g * POOL_GROUP, (g + 1) * POOL_GROUP)
        lo = jnp.maximum(t + 1 - w, 0)
        win_sum = cs[:, 1:, sl] - cs[:, lo, sl]
        cnt = jnp.minimum(t + 1, w).astype(jnp.float32)
        parts.append(win_sum / cnt[None, :, None] - h32[..., sl])
    d = jnp.stack(parts, axis=2).astype(h.dtype)
    y = jnp.einsum('bsgc,gcd->bsgd', d, w_grp).reshape(B, S, D)
    return y * scale


def forgetting_attention(h, w_in, b_f, w_o):
    B, S, D = h.shape
    proj = h @ w_in
    q = proj[..., :D].reshape(B, S, N_HEADS, HEAD_DIM)
    k = proj[..., D:2 * D].reshape(B, S, N_HEADS, HEAD_DIM)
    v = proj[..., 2 * D:3 * D].reshape(B, S, N_HEADS, HEAD_DIM)
    logf = jax.nn.log_sigmoid((proj[..., 3 * D:] + b_f).astype(jnp.float32))
    Ft = jnp.cumsum(logf, axis=1).transpose(0, 2, 1)
    kpos = jnp.arange(S)
    sm_scale = HEAD_DIM ** -0.5

    def attend(qi):
        s0 = qi * Q_BLOCK
        qb = lax.dynamic_slice_in_dim(q, s0, Q_BLOCK, axis=1)
        Fq = lax.dynamic_slice_in_dim(Ft, s0, Q_BLOCK, axis=2)
        logits = jnp.einsum('bqhd,bkhd->bhqk', qb, k, preferred_element_type=jnp.float32) * sm_scale
        logits = logits + Fq[..., :, None] - Ft[:, :, None, :]
        qpos = s0 + jnp.arange(Q_BLOCK)
        mask = qpos[:, None] >= kpos[None, :]
        p = jax.nn.softmax(jnp.where(mask, logits, NEG_INF), axis=-1)
        return jnp.einsum('bhqk,bkhd->bqhd', p.astype(v.dtype), v)

    o = lax.map(attend, jnp.arange(S // Q_BLOCK))
    o = o.transpose(1, 0, 2, 3, 4).reshape(B, S, D)
    return o @ w_o


def moe_ffn(h, router_w, router_b, w_in, b_in, w_out, b_out):
    B, S, D = h.shape
    T = B * S
    TK = T * TOP_K
    ht = h.reshape(T, D)
    logits = (ht @ router_w + router_b).astype(jnp.float32)
    top_v, top_i = lax.top_k(logits, TOP_K)
    gates = jax.nn.softmax(top_v, axis=-1)
    e_flat = top_i.reshape(-1)
    tok_flat = jnp.repeat(jnp.arange(T, dtype=jnp.int32), TOP_K)
    g_flat = gates.reshape(-1)
    order = jnp.argsort(e_flat, stable=True)
    e_sorted = e_flat[order]
    counts = jnp.bincount(e_flat, length=N_EXPERTS)
    padded = ((counts + EXPERT_BLOCK - 1) // EXPERT_BLOCK) * EXPERT_BLOCK
    start = jnp.cumsum(counts) - counts
    pend = jnp.cumsum(padded)
    pstart = pend - padded
    dest = pstart[e_sorted] + jnp.arange(TK) - start[e_sorted]
    n_blocks = TK // EXPERT_BLOCK + N_EXPERTS
    P = n_blocks * EXPERT_BLOCK
    row_tok = jnp.zeros((P,), jnp.int32).at[dest].set(tok_flat[order])
    row_gate = jnp.zeros((P,), jnp.float32).at[dest].set(g_flat[order])
    block_e = jnp.minimum(jnp.searchsorted(pend, jnp.arange(n_blocks) * EXPERT_BLOCK, side='right'), N_EXPERTS - 1)
    xs = ht[row_tok].reshape(n_blocks, EXPERT_BLOCK, D)

    def expert_block(args):
        xb, e = args
        gu = xb @ w_in[e] + b_in[e]
        gate = jnp.minimum(gu[:, :D_EXPERT], SWIGLU_LIMIT)
        up = jnp.clip(gu[:, D_EXPERT:], -SWIGLU_LIMIT, SWIGLU_LIMIT)
        act = (up + 1) * (gate * jax.nn.sigmoid(SWIGLU_ALPHA * gate))
        return act @ w_out[e] + b_out[e]

    ys = lax.map(expert_block, (xs, block_e)).reshape(P, D)
    out = jnp.zeros((T, D), jnp.float32).at[row_tok].add(ys.astype(jnp.float32) * row_gate[:, None])
    return out.astype(h.dtype).reshape(B, S, D)


def setup_inputs(seed: int = 0) -> dict:
    key = jax.random.key(seed)
    ks = jax.random.split(key, 20)
    D, H, E, F = D_MODEL, N_HEADS, N_EXPERTS, D_EXPERT
    nrm = jax.random.normal
    return {
        'x': nrm(ks[0], (BATCH, SEQ, D), jnp.float32),
        'c': nrm(ks[1], (BATCH, D), jnp.float32),
        'norm_mix_g': 1.0 + 0.05 * nrm(ks[2], (DEPTH, D), jnp.float32),
        'norm_ffn_g': 1.0 + 0.05 * nrm(ks[3], (DEPTH, D), jnp.float32),
        'ada_w': 0.5 * D ** -0.5 * nrm(ks[4], (DEPTH, D, 6 * D), jnp.float32),
        'ada_b': 0.02 * nrm(ks[5], (DEPTH, 6 * D), jnp.float32),
        'pool_w': POOL_GROUP ** -0.5 * nrm(ks[6], (N_POOL, N_POOL_GROUPS, POOL_GROUP, POOL_GROUP), jnp.float32),
        'pool_scale': 1.0 + 0.1 * nrm(ks[7], (N_POOL, D), jnp.float32),
        'fox_w_in': D ** -0.5 * nrm(ks[8], (N_FOX, D, 3 * D + H), jnp.float32),
        'fox_b_f': 2.0 + 0.1 * nrm(ks[9], (N_FOX, H), jnp.float32),
        'fox_w_o': D ** -0.5 * nrm(ks[10], (N_FOX, D, D), jnp.float32),
        'router_w': D ** -0.5 * nrm(ks[11], (DEPTH, D, E), jnp.float32),
        'router_b': 0.01 * nrm(ks[12], (DEPTH, E), jnp.float32),
        'exp_w_in': D ** -0.5 * nrm(ks[13], (DEPTH, E, D, 2 * F), jnp.float32),
        'exp_b_in': 0.02 * nrm(ks[14], (DEPTH, E, 2 * F), jnp.float32),
        'exp_w_out': F ** -0.5 * nrm(ks[15], (DEPTH, E, F, D), jnp.float32),
        'exp_b_out': 0.02 * nrm(ks[16], (DEPTH, E, D), jnp.float32),
        'final_g': 1.0 + 0.05 * nrm(ks[17], (D,), jnp.float32),
    }


def reference(x, c, norm_mix_g, norm_ffn_g, ada_w, ada_b, pool_w, pool_scale, fox_w_in, fox_b_f, fox_w_o,
              router_w, router_b, exp_w_in, exp_b_in, exp_w_out, exp_b_out, final_g):
    cond = jax.nn.silu(c)
    for i in range(DEPTH):
        mod = cond @ ada_w[i] + ada_b[i]
        sh1, sc1, g1, sh2, sc2, g2 = jnp.split(mod, 6, axis=-1)
        h = modulate(rmsnorm(x, norm_mix_g[i]), sh1, sc1)
        j = i // N_MIXERS
        if i % N_MIXERS == 0:
            m = pool_mixer(h, pool_w[j], pool_scale[j])
        else:
            m = forgetting_attention(h, fox_w_in[j], fox_b_f[j], fox_w_o[j])
        x = x + g1[:, None, :] * m
        h = modulate(rmsnorm(x, norm_ffn_g[i]), sh2, sc2)
        x = x + g2[:, None, :] * moe_ffn(h, router_w[i], router_b[i], exp_w_in[i], exp_b_in[i], exp_w_out[i], exp_b_out[i])
    return rmsnorm(x, final_g)
```

```python
import numpy as np
import ml_dtypes
import concourse.bass as bass
import concourse.mybir as mybir
from concourse.bass_utils import run_bass_kernel_spmd

F32 = mybir.dt.float32
BF16 = mybir.dt.bfloat16
AF = mybir.ActivationFunctionType
ALU = mybir.AluOpType

ENGS = ("sync", "scalar", "vector", "gpsimd", "tensor")
NCORES = 8
D = 1024
SEQ = 8192
TOK = 4096
NT = TOK // 128
NE = 32
EPS = 1e-6
NKT = SEQ // 128
NQB = SEQ // 512
PAIRS = [[0, 1], [2, 3], [4, 5], [6, 7]]


class Res:
    __slots__ = ("name", "last_write", "reads")

    def __init__(self, name=""):
        self.name = name
        self.last_write = None
        self.reads = {}


class Op:
    __slots__ = ("eng", "fn", "deps", "signal", "dma_sem", "tick", "is_dma")

    def __init__(self, eng, fn, is_dma=False, dma_sem=None):
        self.eng = eng
        self.fn = fn
        self.deps = []
        self.signal = False
        self.is_dma = is_dma
        self.dma_sem = dma_sem
        self.tick = None


class DmaSem:
    def __init__(self, prog, name, inc=16):
        self.sem = prog.new_sem(name)
        self.count = 0
        self.inc = inc
        self.last_op = None
        prog.all_dsems.append(self)


class Prog:
    def __init__(self, nc):
        self.nc = nc
        self.ops = {e: [] for e in ENGS}
        self._sem_ctx = []
        self.eng_sem = {}
        self.all_dsems = []
        self.scr = None

    def new_sem(self, name):
        ctx = self.nc.semaphore(name)
        s = ctx.__enter__()
        self._sem_ctx.append(ctx)
        return s

    def _add(self, eng, fn, reads, writes, is_dma=False, dma_sem=None, extra_deps=()):
        op = Op(eng, fn, is_dma, dma_sem)
        self.ops[eng].append(op)
        deps = list(extra_deps)
        for r in reads:
            if r.last_write is not None:
                deps.append(r.last_write)
        for w in writes:
            if w.last_write is not None:
                deps.append(w.last_write)
            deps.extend(w.reads.values())
        seen = set()
        for d in deps:
            if d is op or id(d) in seen:
                continue
            seen.add(id(d))
            if d.eng == eng and not d.is_dma and eng == "tensor":
                continue
            op.deps.append(d)
            d.signal = True
        for r in reads:
            r.reads[eng] = op
        for w in writes:
            w.last_write = op
            w.reads = {}
        return op

    def op(self, eng, fn, reads=(), writes=(), extra_deps=()):
        return self._add(eng, fn, reads, writes, extra_deps=extra_deps)

    def dma(self, eng, fn, dsem, reads=(), writes=(), extra_deps=()):
        op = self._add(eng, fn, reads, writes, is_dma=True, dma_sem=dsem, extra_deps=extra_deps)
        dsem.count += dsem.inc
        op.tick = dsem.count
        op.signal = True
        dsem.last_op = op
        return op

    def barrier(self, pe_out, pe_res):
        nc = self.nc
        if self.scr is None:
            self.scr = {e: nc.alloc_sbuf_tensor("bar_scr_" + e, [128, 16], F32) for e in ENGS}
            self.scrb = nc.alloc_sbuf_tensor("bar_scrb", [128, 16], BF16)
            self.bres = {e: Res("bar_" + e) for e in ENGS}
            self.bres2 = {e: Res("bar2_" + e) for e in ENGS}
            self.bsem = DmaSem(self, "bar_dma")
            self.bsem2 = DmaSem(self, "bar_dma2")
            self.op("vector", lambda e: e.memset(self.scrb[:], 0.0), writes=[self.bres["tensor"]])
            self.op("vector", lambda e: e.memset(self.scr["sync"][:], 0.0), writes=[self.bres["sync"]])
            self.op("vector", lambda e: e.memset(self.scr["scalar"][:], 0.0), writes=[self.bres["scalar"]])
        scr = self.scr

        def tiny(eng, stage):
            c0 = stage * 4
            if eng == "vector":
                return lambda e: e.memset(scr["vector"][0:1, c0:c0 + 1], 0.0)
            if eng == "gpsimd":
                return lambda e: e.memset(scr["gpsimd"][0:1, c0:c0 + 1], 0.0)
            if eng == "scalar":
                return lambda e: e.activation(out=scr["scalar"][0:1, c0:c0 + 1], in_=scr["scalar"][0:1, 8:9], func=AF.Copy)
            if eng == "tensor":
                return lambda e: e.matmul(pe_out[0:1, 0:1], lhsT=self.scrb[0:1, 0:1], rhs=self.scrb[0:1, 0:1], start=True, stop=True)
            return None

        bops = []
        for eng in ENGS:
            if eng == "sync":
                o = self.dma("sync", lambda e: e.dma_start(out=scr["sync"][0:1, 0:4], in_=scr["sync"][0:1, 8:12]), self.bsem,
                             writes=[self.bres[eng]])
            elif eng == "tensor":
                o = self.op(eng, tiny(eng, 0), reads=[self.bres[eng]], writes=[pe_res])
            else:
                o = self.op(eng, tiny(eng, 0), writes=[self.bres[eng]])
            o.signal = True
            bops.append(o)
        outstanding = [ds.last_op for ds in self.all_dsems if ds.last_op is not None]
        for eng in ENGS:
            deps = bops + outstanding
            if eng == "sync":
                self.dma("sync", lambda e: e.dma_start(out=scr["sync"][0:1, 4:8], in_=scr["sync"][0:1, 12:16]), self.bsem2,
                         writes=[self.bres2[eng]], extra_deps=deps)
            elif eng == "tensor":
                self.op(eng, tiny(eng, 1), writes=[pe_res], extra_deps=deps)
            else:
                self.op(eng, tiny(eng, 1), writes=[self.bres2[eng]], extra_deps=deps)

    def emit(self, final_waits=()):
        nc = self.nc
        for e in ENGS:
            self.eng_sem[e] = self.new_sem("eng_" + e)
        for e in ENGS:
            t = 0
            for op in self.ops[e]:
                if op.is_dma:
                    continue
                if op.signal:
                    t += 1
                    op.tick = t
        prog = self

        def chan(op):
            return op.dma_sem.sem if op.is_dma else prog.eng_sem[op.eng]

        def replay(engname, e, extra_final=()):
            waited = {}

            def do_wait(d):
                s = chan(d)
                k = id(s)
                if waited.get(k, 0) >= d.tick:
                    return
                e.wait_ge(s, d.tick)
                waited[k] = d.tick

            for op in prog.ops[engname]:
                for d in op.deps:
                    do_wait(d)
                ins = op.fn(e)
                if op.is_dma:
                    if op.dma_sem.inc == 16:
                        ins.then_inc(op.dma_sem.sem, 16)
                    else:
                        ins.then_inc(op.dma_sem.sem)
                elif op.signal:
                    ins.then_inc(prog.eng_sem[engname], 1)
            for d in extra_final:
                do_wait(d)

        with nc.Block() as block:
            @block.sync
            def _(e):
                replay("sync", e, final_waits)

            @block.scalar
            def _(e):
                replay("scalar", e)

            @block.vector
            def _(e):
                replay("vector", e)

            @block.gpsimd
            def _(e):
                replay("gpsimd", e)

            @block.tensor
            def _(e):
                replay("tensor", e)

    def close(self):
        for ctx in reversed(self._sem_ctx):
            ctx.__exit__(None, None, None)
        self._sem_ctx = []


class Ring:
    def __init__(self, C, name, n, shape, dtype):
        self.t = [C.A("%s%d" % (name, i), shape, dtype) for i in range(n)]
        self.r = [Res("%s%d" % (name, i)) for i in range(n)]
        self.n = n
        self.i = -1

    def next(self):
        self.i = (self.i + 1) % self.n
        return self.t[self.i], self.r[self.i]


_DTSIZE = {F32: 4, BF16: 2}


class Ctx:
    def __init__(self, nc):
        self.nc = nc
        self.P = Prog(nc)
        self.PB = [nc.alloc_psum_tensor("pb%d" % i, [128, 512], F32) for i in range(8)]
        self.RB = [Res("pb%d" % i) for i in range(8)]
        self.pool = []
        self.pool_i = 0
        self.cpool = []
        self.cpool_i = 0
        self.uid = 0
        self.arena_base = None
        self.arena_size = 0
        self.off = 0

    def finalize_globals(self):
        nc = self.nc
        size = (nc.sbuf_bytes_remaining - 2048) // 64 * 64
        arena = nc.alloc_sbuf_tensor("arena", [128, size // 4], F32)
        self.arena_base = nc.lookup_mloc(arena).addr
        self.arena_size = size

    def A(self, name, shape, dtype):
        n = _DTSIZE[dtype]
        for s in shape[1:]:
            n *= s
        n = (n + 63) // 64 * 64
        assert self.off + n <= self.arena_size, ("SBUF arena overflow", name, self.off, n, self.arena_size)
        self.uid += 1
        h = self.nc.alloc_sbuf_tensor_at("%s_u%d" % (name, self.uid), list(shape), dtype, offset=self.arena_base + self.off)
        self.off += n
        return h

    def ring(self, name, n, shape, dtype):
        return Ring(self, name, n, shape, dtype)

    def dsem(self):
        if self.pool_i == len(self.pool):
            self.pool.append(DmaSem(self.P, "ds%d" % len(self.pool)))
        s = self.pool[self.pool_i]
        self.pool_i += 1
        return s

    def csem(self):
        if self.cpool_i == len(self.cpool):
            self.cpool.append(DmaSem(self.P, "cs%d" % len(self.cpool), inc=1))
        s = self.cpool[self.cpool_i]
        self.cpool_i += 1
        return s

    def phase(self):
        self.P.barrier(self.PB[7], self.RB[7])
        self.off = 0
        self.pool_i = 0
        self.cpool_i = 0


def _din(nc, name, shape, dt=F32):
    return nc.dram_tensor(name, list(shape), dt, kind="ExternalInput").ap()


def _dout(nc, name, shape, dt=F32):
    return nc.dram_tensor(name, list(shape), dt, kind="ExternalOutput").ap()


def emit_rstd(P, xt, xr, junk, junkr, ss, sd, rstd, sr):
    P.op("scalar", lambda e: e.activation(out=junk[:], in_=xt, func=AF.Square, accum_out=ss[:]),
         reads=[xr], writes=[junkr, sr])
    P.op("scalar", lambda e: e.activation(out=sd[:], in_=ss[:], func=AF.Sqrt, scale=1.0 / D, bias=EPS),
         reads=[sr], writes=[sr])
    P.op("vector", lambda e: e.reciprocal(out=rstd[:], in_=sd[:]), reads=[sr], writes=[sr])


def emit_M(C, cT, adaw, adab, ng, modrows):
    P, PB, RB = C.P, C.PB, C.RB
    c_sb = C.A("c_sb", [128, 8], F32); r_c = Res()
    cond = C.A("cond", [128, 8], F32); r_cond = Res()
    adab_r = C.ring("adab_sb", 2, [1, 6 * D], F32)
    ng_r = C.ring("ng_sb", 2, [1, 2 * D], F32)
    mod_r = C.ring("mod_sb", 2, [1, 6 * D], F32)
    wch = C.ring("wch", 2, [128, 8, 512], F32)
    s_w = [C.dsem() for _ in range(2)]
    s_ab = [C.dsem() for _ in range(2)]
    s_ng = [C.dsem() for _ in range(2)]
    s_mo = [C.dsem() for _ in range(2)]
    P.dma("sync", lambda e: e.dma_start(out=c_sb[:], in_=cT[:, :]), C.dsem(), writes=[r_c])
    P.op("scalar", lambda e: e.activation(out=cond[:], in_=c_sb[:], func=AF.Silu), reads=[r_c], writes=[r_cond])
    i = 0
    for li in range(4):
        abt, abr = adab_r.next()
        ngt, ngr = ng_r.next()
        mt, mr = mod_r.next()
        sl = adab_r.i
        P.dma("sync", lambda e, abt=abt, li=li: e.dma_start(out=abt[:], in_=adab[0:1, li * 6 * D:(li + 1) * 6 * D]), s_ab[sl], writes=[abr])
        P.dma("sync", lambda e, ngt=ngt, li=li: e.dma_start(out=ngt[:], in_=ng[0:1, li * 2 * D:(li + 1) * 2 * D]), s_ng[sl], writes=[ngr])
        for nch in range(12):
            wt, wr = wch.next()
            P.dma("sync", lambda e, wt=wt, li=li, nch=nch: e.dma_start(
                out=wt[:], in_=adaw[li, :, nch * 512:(nch + 1) * 512].rearrange("(k p) n -> p k n", p=128)),
                s_w[wch.i], writes=[wr])
            b = i % 2
            i += 1
            for k in range(8):
                P.op("tensor", lambda e, k=k, b=b, wt=wt: e.matmul(PB[b][0:1, :], lhsT=cond[:, k:k + 1], rhs=wt[:, k, :],
                                                                  start=(k == 0), stop=(k == 7)),
                     reads=[r_cond, wr], writes=[RB[b]])
            c0 = nch * 512
            P.op("vector", lambda e, b=b, c0=c0, mt=mt, abt=abt: e.tensor_tensor(out=mt[0:1, c0:c0 + 512], in0=PB[b][0:1, :],
                                                                                in1=abt[0:1, c0:c0 + 512], op=ALU.add),
                 reads=[RB[b], abr], writes=[mr])
        for (v, gi) in ((1, 0), (4, 1)):
            c0 = v * D
            g0 = gi * D
            P.op("vector", lambda e, c0=c0, g0=g0, mt=mt, ngt=ngt: e.scalar_tensor_tensor(
                out=mt[0:1, c0:c0 + D], in0=mt[0:1, c0:c0 + D], scalar=1.0, in1=ngt[0:1, g0:g0 + D], op0=ALU.add, op1=ALU.mult),
                 reads=[mr, ngr], writes=[mr])
        P.dma("sync", lambda e, mt=mt, li=li: e.dma_start(out=modrows[0:1, li * 6 * D:(li + 1) * 6 * D], in_=mt[:]), s_mo[sl], reads=[mr])


def emit_modT(C, modrows, modT_sb, r_modT):
    P = C.P
    for lv in range(24):
        P.dma("sync", lambda e, lv=lv: e.dma_start(out=modT_sb[:, lv * 8:(lv + 1) * 8],
                                                   in_=modrows[0, lv * D:(lv + 1) * D].rearrange("(k p) -> p k", p=128),
                                                   allow_slow_non_contiguous=True),
              C.dsem(), writes=[r_modT])


def emit_tail(C, xsrc, tail_loc, tail_g):
    P = C.P
    r_t = Res()
    P.dma("gpsimd", lambda e: e.dma_start(out=tail_loc[:, :], in_=xsrc[TOK - 128:TOK, :]), C.dsem(), writes=[r_t])
    P.dma("gpsimd", lambda e: e.collective_compute("AllGather", ALU.bypass, replica_groups=PAIRS,
                                                   ins=[tail_loc[:, :]], outs=[tail_g[:, :]]), C.csem(), reads=[r_t])


def emit_Pm(C, layer, xsrc, xdst, xh, modrows, pw, pscale, poolA):
    P, PB, RB = C.P, C.PB, C.RB
    a1bc = C.A("a1bc", [128, D], F32); sh1bc = C.A("sh1bc", [128, D], F32); r_bc = Res()
    g1bc = C.A("g1bc", [128, D], F32); psbc = C.A("psbc", [128, D], F32); r_s = Res()
    pwf = C.A("pwf", [128, 8, 256], F32); r_pwf = Res()
    Wb = C.A("Wb", [128, 8, 256], BF16); r_Wb = Res()
    Af = C.A("Af", [128, 16, 128], F32); r_Af = Res()
    Abf = C.A("Abf", [128, 16, 128], BF16); r_Ab = Res()
    xt = C.ring("xt", 3, [128, D], F32)
    tmp = C.ring("tmp", 2, [128, D], F32)
    hr = C.ring("hr", 3, [128, D], BF16)
    junk = C.A("junk", [128, D], BF16); r_junk = Res()
    st = C.ring("st", 2, [128, 4], F32)
    dT = C.ring("dT", 2, [128, 8, 128], BF16)
    s_x = [C.dsem() for _ in range(3)]
    s_o = [C.dsem() for _ in range(3)]
    r0 = layer * 6
    P.dma("sync", lambda e: e.dma_start(out=sh1bc[:], in_=modrows[0, (r0 + 0) * D:(r0 + 1) * D].partition_broadcast(128)), C.dsem(), writes=[r_bc])
    P.dma("sync", lambda e: e.dma_start(out=a1bc[:], in_=modrows[0, (r0 + 1) * D:(r0 + 2) * D].partition_broadcast(128)), C.dsem(), writes=[r_bc])
    P.dma("sync", lambda e: e.dma_start(out=g1bc[:], in_=modrows[0, (r0 + 2) * D:(r0 + 3) * D].partition_broadcast(128)), C.dsem(), writes=[r_s])
    P.dma("sync", lambda e: e.dma_start(out=psbc[:], in_=pscale[0, :].partition_broadcast(128)), C.dsem(), writes=[r_s])
    P.dma("sync", lambda e: e.dma_start(out=pwf[:], in_=pw.rearrange("g (kk p) n -> p (g kk) n", p=128)), C.dsem(), writes=[r_pwf])
    P.dma("sync", lambda e: e.dma_start(out=Af[:], in_=poolA.rearrange("i p n -> p i n")), C.dsem(), writes=[r_Af])
    P.op("vector", lambda e: e.tensor_tensor(out=g1bc[:], in0=g1bc[:], in1=psbc[:], op=ALU.mult), reads=[r_s], writes=[r_s])
    for k in range(8):
        g = k // 2
        P.op("vector", lambda e, k=k, g=g: e.tensor_tensor(out=Wb[:, k, :], in0=pwf[:, k, :], in1=g1bc[:, g * 256:(g + 1) * 256],
                                                           op=ALU.mult), reads=[r_pwf, r_s], writes=[r_Wb])
    P.op("vector", lambda e: e.tensor_copy(out=Abf[:], in_=Af[:]), reads=[r_Af], writes=[r_Ab])

    def make_h(src_ap, xtt, xtr, slot):
        P.dma("sync", lambda e: e.dma_start(out=xtt[:], in_=src_ap), s_x[slot], writes=[xtr])
        stt, str_ = st.next()
        emit_rstd(P, xtt[:], xtr, junk, r_junk, stt[:, 0:1], stt[:, 1:2], stt[:, 2:3], str_)
        tt, tr = tmp.next()
        P.op("vector", lambda e: e.scalar_tensor_tensor(out=tt[:], in0=xtt[:], scalar=stt[:, 2:3], in1=a1bc[:],
                                                        op0=ALU.mult, op1=ALU.mult), reads=[xtr, str_, r_bc], writes=[tr])
        ht, hrr = hr.next()
        P.op("gpsimd", lambda e: e.tensor_tensor(out=ht[:], in0=tt[:], in1=sh1bc[:], op=ALU.add), reads=[tr, r_bc], writes=[hrr])
        return ht, hrr

    xtt, xtr = xt.next()
    hprev, hprev_r = make_h(xh, xtt, xtr, xt.i)
    for t in range(NT):
        xtt, xtr = xt.next()
        slot = xt.i
        hcur, hcur_r = make_h(xsrc[t * 128:(t + 1) * 128, :], xtt, xtr, slot)
        kc, kp = (2, 3) if t == 0 else (0, 1)
        pd = (t % 2) * 2
        for k in range(8):
            g = k // 2
            b = pd + k // 4
            cs = slice((k % 4) * 128, (k % 4 + 1) * 128)
            P.op("tensor", lambda e, k=k, b=b, cs=cs, g=g, hp=hprev, kp=kp: e.matmul(
                PB[b][:, cs], lhsT=hp[:, k * 128:(k + 1) * 128], rhs=Abf[:, kp * 4 + g, :], start=True, stop=False),
                reads=[hprev_r, r_Ab], writes=[RB[b]])
            P.op("tensor", lambda e, k=k, b=b, cs=cs, g=g, hc=hcur, kc=kc: e.matmul(
                PB[b][:, cs], lhsT=hc[:, k * 128:(k + 1) * 128], rhs=Abf[:, kc * 4 + g, :], start=False, stop=True),
                reads=[hcur_r, r_Ab], writes=[RB[b]])
        dt_, dr = dT.next()
        for hb in range(2):
            P.op("scalar", lambda e, hb=hb, pd=pd, dt_=dt_: e.activation(
                out=dt_[:, hb * 4:(hb + 1) * 4, :], in_=PB[pd + hb][:].rearrange("p (k t) -> p k t", k=4), func=AF.Copy),
                reads=[RB[pd + hb]], writes=[dr])
        py = 4 + (t % 2) * 2
        for g in range(4):
            b = py + g // 2
            cs = slice((g % 2) * 256, (g % 2 + 1) * 256)
            for kk in range(2):
                k = 2 * g + kk
                P.op("tensor", lambda e, k=k, b=b, cs=cs, kk=kk, dt_=dt_: e.matmul(
                    PB[b][:, cs], lhsT=dt_[:, k, :], rhs=Wb[:, k, :], start=(kk == 0), stop=(kk == 1)),
                    reads=[dr, r_Wb], writes=[RB[b]])
        for hb in range(2):
            cs = slice(hb * 512, (hb + 1) * 512)
            P.op("vector", lambda e, hb=hb, cs=cs, py=py, xtt=xtt: e.tensor_tensor(out=xtt[:, cs], in0=PB[py + hb][:], in1=xtt[:, cs],
                                                                                 op=ALU.add),
                 reads=[RB[py + hb], xtr], writes=[xtr])
        P.dma("sync", lambda e, t=t, xtt=xtt: e.dma_start(out=xdst[t * 128:(t + 1) * 128, :], in_=xtt[:]), s_o[slot], reads=[xtr])
        hprev, hprev_r = hcur, hcur_r


def emit_Aa(C, layer, xd, hT_loc, modT_sb, r_modT, idf, r_id):
    P, PB, RB = C.P, C.PB, C.RB
    xt = C.ring("xt", 3, [128, D], F32)
    xn = C.ring("xn", 2, [128, D], F32)
    junk = C.A("junk", [128, D], BF16); r_junk = Res()
    st = C.ring("st", 2, [128, 4], F32)
    ht = C.ring("ht", 3, [128, 8, 128], BF16)
    s_x = [C.dsem() for _ in range(3)]
    s_o = [C.dsem() for _ in range(3)]
    m0 = layer * 48
    hTv = hT_loc.rearrange("k p t -> p k t")
    for t in range(NT):
        xtt, xtr = xt.next()
        P.dma("sync", lambda e, t=t, xtt=xtt: e.dma_start(out=xtt[:], in_=xd[t * 128:(t + 1) * 128, :]), s_x[xt.i], writes=[xtr])
        stt, str_ = st.next()
        emit_rstd(P, xtt[:], xtr, junk, r_junk, stt[:, 0:1], stt[:, 1:2], stt[:, 2:3], str_)
        xnt, xnr = xn.next()
        P.op("vector", lambda e, xnt=xnt, xtt=xtt, stt=stt: e.tensor_scalar(out=xnt[:], in0=xtt[:], scalar1=stt[:, 2:3], scalar2=None,
                                                                          op0=ALU.mult), reads=[xtr, str_], writes=[xnr])
        pd = (t % 2) * 2
        for k in range(8):
            b = pd + k // 4
            P.op("tensor", lambda e, k=k, b=b, xnt=xnt: e.transpose(out=PB[b][:, (k % 4) * 128:(k % 4 + 1) * 128],
                                                                   in_=xnt[:, k * 128:(k + 1) * 128], identity=idf[:]),
                 reads=[xnr, r_id], writes=[RB[b]])
        htt, htr = ht.next()
        for k in range(8):
            b = pd + k // 4
            P.op("scalar", lambda e, k=k, b=b, htt=htt: e.activation(out=htt[:, k, :], in_=PB[b][:, (k % 4) * 128:(k % 4 + 1) * 128],
                                                                    func=AF.Identity, scale=modT_sb[:, m0 + 8 + k:m0 + 8 + k + 1],
                                                                    bias=modT_sb[:, m0 + k:m0 + k + 1]),
                 reads=[RB[b], r_modT], writes=[htr])
        P.dma("sync", lambda e, t=t, htt=htt: e.dma_start(out=hTv[:, :, t * 128:(t + 1) * 128], in_=htt[:]), s_o[ht.i], reads=[htr])


def emit_gather8(C, loc, gth):
    P = C.P
    for j in range(8):
        P.dma("gpsimd", lambda e, j=j: e.collective_compute("AllGather", ALU.bypass, replica_groups=PAIRS,
                                                            ins=[loc[j]], outs=[gth[j]]), C.csem())


def emit_Ab(C, j, hT_g, wq, wk, wv, wf, bfr, tri, sel, maskb, o_loc, idf, r_id, nhp=4, nqb=NQB):
    P, PB, RB = C.P, C.PB, C.RB
    wq_b = C.A("wq_b", [128, 8, 512], BF16); wk_b = C.A("wk_b", [128, 8, 512], BF16); wv_b = C.A("wv_b", [128, 8, 512], BF16)
    wf_b = C.A("wf_b", [128, 8, 8], BF16); r_w = Res()
    bf_sb = C.A("bf_sb", [128, 8], F32); nbf = C.A("nbf", [128, 8], F32); r_bf = Res()
    tri_sb = C.A("tri_sb", [128, 128], F32); sel_sb = C.A("sel_sb", [128, 128], F32)
    ones_sb = C.A("ones_sb", [128, 128], F32); r_cst = Res()
    mk = C.A("mk", [128, 4, 512], F32); r_mk = Res()
    qT = C.A("qT", [128, SEQ], BF16); r_qT = [Res() for _ in range(NQB)]
    kT = C.A("kT", [128, SEQ], BF16); r_kT = [Res() for _ in range(NQB)]
    V = C.A("V", [128, NKT, 2, 65], BF16); r_V = [Res() for _ in range(NQB)]
    NF = C.A("NF", [128, NKT, 2], F32); r_NF = [Res() for _ in range(NKT)]
    hblk = C.ring("hblk", 2, [128, 8, 512], BF16)
    e1 = C.ring("e1", 2, [128, 16], F32)
    dg = C.ring("dg", 2, [128, 128], F32)
    mnf = C.ring("mnf", 2, [128, 512], F32)
    comb = C.ring("comb", 2, [128, 4, 512], F32)
    tt = C.ring("tt", 6, [128, 512], F32)
    pT = C.ring("pT", 6, [128, 512], BF16)
    rec = C.ring("rec", 2, [128, 4], F32)
    ost = C.ring("ost", 2, [128, 4, 128], BF16)
    s_h = [C.dsem() for _ in range(2)]
    s_o = [C.dsem() for _ in range(2)]
    for (dst, src) in ((wq_b, wq[j]), (wk_b, wk[j]), (wv_b, wv[j]), (wf_b, wf[j])):
        P.dma("gpsimd", lambda e, dst=dst, src=src: e.dma_start(out=dst[:], in_=src.rearrange("(k p) n -> p k n", p=128)),
              C.dsem(), writes=[r_w])
    P.dma("sync", lambda e: e.dma_start(out=bf_sb[:], in_=bfr[0, j * 8:(j + 1) * 8].partition_broadcast(128)), C.dsem(), writes=[r_bf])
    for (dst, src) in ((tri_sb, tri), (sel_sb, sel)):
        P.dma("sync", lambda e, dst=dst, src=src: e.dma_start(out=dst[:], in_=src[:, :]), C.dsem(), writes=[r_cst])
    P.dma("sync", lambda e: e.dma_start(out=mk[:], in_=maskb.rearrange("r p n -> p r n")), C.dsem(), writes=[r_mk])
    P.op("vector", lambda e: e.memset(ones_sb[:], 1.0), writes=[r_cst])
    P.op("vector", lambda e: e.tensor_scalar(out=nbf[:], in0=bf_sb[:], scalar1=-1.0, scalar2=None, op0=ALU.mult),
         reads=[r_bf], writes=[r_bf])
    P.op("vector", lambda e: e.memset(V[:, :, :, 64:65], 1.0), writes=r_V)
    obi = [0]
    sring = [0]
    for hp in range(nhp):
        hs = slice(hp * 128, (hp + 1) * 128)
        for tb in range(NQB):
            ts_ = slice(tb * 512, (tb + 1) * 512)
            rk = tb // 8
            lo = (tb % 8) * 512
            hb, hbr = hblk.next()
            P.dma("sync", lambda e, hb=hb, rk=rk, lo=lo: e.dma_start(
                out=hb[:], in_=hT_g[:, rk * 128:(rk + 1) * 128, lo:lo + 512].rearrange("k p t -> p k t")), s_h[hblk.i], writes=[hbr])
            for k in range(8):
                P.op("tensor", lambda e, k=k, hb=hb, hs=hs: e.matmul(PB[0][:], lhsT=wq_b[:, k, hs], rhs=hb[:, k, :],
                                                                    start=(k == 0), stop=(k == 7)), reads=[r_w, hbr], writes=[RB[0]])
            P.op("scalar", lambda e, ts_=ts_: e.activation(out=qT[:, ts_], in_=PB[0][:], func=AF.Copy, scale=0.125),
                 reads=[RB[0]], writes=[r_qT[tb]])
            for k in range(8):
                P.op("tensor", lambda e, k=k, hb=hb, hs=hs: e.matmul(PB[1][:], lhsT=wk_b[:, k, hs], rhs=hb[:, k, :],
                                                                    start=(k == 0), stop=(k == 7)), reads=[r_w, hbr], writes=[RB[1]])
            P.op("vector", lambda e, ts_=ts_: e.tensor_copy(out=kT[:, ts_], in_=PB[1][:]), reads=[RB[1]], writes=[r_kT[tb]])
            for t4 in range(4):
                cs = slice(t4 * 128, (t4 + 1) * 128)
                for k in range(8):
                    P.op("tensor", lambda e, k=k, hb=hb, hs=hs, cs=cs: e.matmul(PB[2][:, cs], lhsT=hb[:, k, cs], rhs=wv_b[:, k, hs],
                                                                               start=(k == 0), stop=(k == 7)),
                         reads=[r_w, hbr], writes=[RB[2]])
            P.op("scalar", lambda e, tb=tb: e.activation(out=V[:, tb * 4:(tb + 1) * 4, :, 0:64],
                                                         in_=PB[2][:].rearrange("p (t h d) -> p t h d", t=4, h=2), func=AF.Copy),
                 reads=[RB[2]], writes=[r_V[tb]])
            for t4 in range(4):
                cs = slice(t4 * 128, (t4 + 1) * 128)
                for k in range(8):
                    P.op("tensor", lambda e, k=k, hb=hb, cs=cs, t4=t4, hp=hp: e.matmul(
                        PB[3][:, t4 * 2:(t4 + 1) * 2], lhsT=hb[:, k, cs], rhs=wf_b[:, k, hp * 2:(hp + 1) * 2], start=(k == 0), stop=(k == 7)),
                        reads=[r_w, hbr], writes=[RB[3]])
            e1t, e1r = e1.next()
            for h2 in range(2):
                P.op("scalar", lambda e, h2=h2, e1t=e1t, hp=hp: e.activation(
                    out=e1t[:, 0:8].rearrange("p (t h) -> p t h", h=2)[:, :, h2:h2 + 1],
                    in_=PB[3][:, 0:8].rearrange("p (t h) -> p t h", h=2)[:, :, h2:h2 + 1],
                    func=AF.Exp, scale=-1.0, bias=nbf[:, hp * 2 + h2:hp * 2 + h2 + 1]),
                    reads=[RB[3], r_bf], writes=[e1r])
            P.op("scalar", lambda e, e1t=e1t: e.activation(out=e1t[:, 8:16], in_=e1t[:, 0:8], func=AF.Ln, bias=1.0, scale=1.0),
                 reads=[e1r], writes=[e1r])
            for t4 in range(4):
                kt = tb * 4 + t4
                cc = slice(16 + t4 * 2, 16 + (t4 + 1) * 2)
                first = (kt == 0)
                P.op("tensor", lambda e, e1t=e1t, t4=t4, cc=cc, first=first: e.matmul(
                    PB[3][:, cc], lhsT=tri_sb[:], rhs=e1t[:, 8 + t4 * 2:8 + (t4 + 1) * 2], start=True, stop=first),
                    reads=[r_cst, e1r], writes=[RB[3]])
                if not first:
                    P.op("tensor", lambda e, kt=kt, cc=cc: e.matmul(PB[3][:, cc], lhsT=sel_sb[:], rhs=NF[:, kt - 1, :], start=False, stop=True),
                         reads=[r_cst, r_NF[kt - 1]], writes=[RB[3]])
                P.op("vector", lambda e, kt=kt, cc=cc: e.tensor_copy(out=NF[:, kt, :], in_=PB[3][:, cc]), reads=[RB[3]], writes=[r_NF[kt]])
        for qb in range(nqb):
            qs = slice(qb * 512, (qb + 1) * 512)
            ostt, ostr = ost.next()
            for h2 in range(2):
                rows = slice(h2 * 64, (h2 + 1) * 64)
                for t4 in range(4):
                    qt = qb * 4 + t4
                    dgt, dgr = dg.next()
                    P.op("vector", lambda e, dgt=dgt, qt=qt, h2=h2: e.tensor_scalar(out=dgt[:], in0=idf[:], scalar1=NF[:, qt, h2:h2 + 1],
                                                                                  scalar2=None, op0=ALU.mult),
                         reads=[r_id, r_NF[qt]], writes=[dgr])
                    P.op("tensor", lambda e, dgt=dgt, t4=t4: e.matmul(PB[3][:, t4 * 128:(t4 + 1) * 128], lhsT=ones_sb[:], rhs=dgt[:],
                                                                     start=True, stop=True), reads=[r_cst, dgr], writes=[RB[3]])
                mt, mr = mnf.next()
                P.op("scalar", lambda e, mt=mt: e.activation(out=mt[:], in_=PB[3][:], func=AF.Copy, scale=-1.0), reads=[RB[3]], writes=[mr])
                ct, cr = comb.next()
                for r in range(4):
                    P.op("gpsimd", lambda e, ct=ct, mt=mt, r=r: e.tensor_tensor(out=ct[:, r, :], in0=mt[:], in1=mk[:, r, :], op=ALU.add),
                         reads=[mr, r_mk], writes=[cr])
                ob = 4 + (obi[0] % 2)
                obi[0] += 1
                nkt = 4 * qb + 4
                sbank = {}

                def issue_S(kt):
                    sb = (0, 1, 2, 6, 7)[sring[0] % 5]
                    sring[0] += 1
                    sbank[kt] = sb
                    P.op("tensor", lambda e, sb=sb, kt=kt, rows=rows, qs=qs: e.matmul(
                        PB[sb][:], lhsT=kT[rows, kt * 128:(kt + 1) * 128], rhs=qT[rows, qs], start=True, stop=True),
                        reads=[r_kT[kt // 4], r_qT[qb]], writes=[RB[sb]])

                LA = 4
                for k0 in range(min(LA, nkt)):
                    issue_S(k0)
                for kt in range(nkt):
                    if kt + LA < nkt:
                        issue_S(kt + LA)
                    sb = sbank[kt]
                    r = kt - 4 * qb
                    ttt, ttr = tt.next()
                    if r >= 0:
                        P.op("vector", lambda e, ttt=ttt, sb=sb, ct=ct, r=r: e.tensor_tensor(out=ttt[:], in0=PB[sb][:], in1=ct[:, r, :], op=ALU.add),
                             reads=[RB[sb], cr], writes=[ttr])
                    else:
                        P.op("vector", lambda e, ttt=ttt, sb=sb, mt=mt: e.tensor_tensor(out=ttt[:], in0=PB[sb][:], in1=mt[:], op=ALU.add),
                             reads=[RB[sb], mr], writes=[ttr])
                    ptt, ptr = pT.next()
                    P.op("scalar", lambda e, ptt=ptt, ttt=ttt, kt=kt, h2=h2: e.activation(out=ptt[:], in_=ttt[:], func=AF.Exp,
                                                                                        bias=NF[:, kt, h2:h2 + 1], scale=1.0),
                         reads=[ttr, r_NF[kt]], writes=[ptr])
                    for c in range(4):
                        if r > c:
                            continue
                        P.op("tensor", lambda e, ptt=ptt, c=c, ob=ob, kt=kt, h2=h2, qb=qb: e.matmul(
                            PB[ob][:, c * 65:(c + 1) * 65], lhsT=ptt[:, c * 128:(c + 1) * 128], rhs=V[:, kt, h2, :],
                            start=(kt == 0 and c == 0), stop=(kt == 4 * qb + c), skip_group_check=True),
                            reads=[ptr, r_V[kt // 4]], writes=[RB[ob]])
                rt, rr = rec.next()
                P.op("vector", lambda e, rt=rt, ob=ob: e.reciprocal(
                    out=rt[:], in_=PB[ob][:, 0:260].rearrange("p (c d) -> p c d", d=65)[:, :, 64]), reads=[RB[ob]], writes=[rr])
                for c in range(4):
                    P.op("vector", lambda e, rt=rt, ob=ob, c=c, h2=h2, ostt=ostt: e.tensor_scalar(
                        out=ostt[:, c, h2 * 64:(h2 + 1) * 64], in0=PB[ob][:, c * 65:c * 65 + 64], scalar1=rt[:, c:c + 1], scalar2=None,
                        op0=ALU.mult), reads=[RB[ob], rr], writes=[ostr])
            P.dma("sync", lambda e, ostt=ostt, qb=qb, hs=hs: e.dma_start(
                out=o_loc[qb // 2, (qb % 2) * 512:(qb % 2 + 1) * 512, hs].rearrange("(c p) d -> p c d", p=128), in_=ostt[:]),
                s_o[ost.i], reads=[ostr])


def emit_Ac(C, layer, j, xd, o_g, wo, modrows, hcol_sb, r_hcol, idf, r_id):
    P, PB, RB = C.P, C.PB, C.RB
    g1bc = C.A("g1bc", [128, D], F32); r_g1 = Res()
    wof = C.A("wof", [128, 8, D], F32); r_wof = Res()
    wob = C.A("wob", [128, 8, D], BF16); r_wob = Res()
    xt = C.ring("xt", 3, [128, D], F32)
    c0 = C.ring("c0", 2, [128, D], BF16)
    c1 = C.ring("c1", 2, [128, D], BF16)
    of = C.ring("of", 2, [128, D], F32)
    oT = C.ring("oT", 2, [128, 8, 128], BF16)
    s_x = [C.dsem() for _ in range(3)]
    s_c0 = [[C.dsem() for _ in range(2)] for _ in range(2)]
    s_c1 = [[C.dsem() for _ in range(2)] for _ in range(2)]
    s_o = [C.dsem() for _ in range(3)]
    r0 = layer * 6
    P.dma("sync", lambda e: e.dma_start(out=g1bc[:], in_=modrows[0, (r0 + 2) * D:(r0 + 3) * D].partition_broadcast(128)), C.dsem(), writes=[r_g1])
    P.dma("sync", lambda e: e.dma_start(out=wof[:], in_=wo[j].rearrange("(k p) n -> p k n", p=128)), C.dsem(), writes=[r_wof])
    for k in range(8):
        P.op("vector", lambda e, k=k: e.tensor_tensor(out=wob[:, k, :], in0=wof[:, k, :], in1=g1bc[:], op=ALU.mult),
             reads=[r_wof, r_g1], writes=[r_wob])
    for t in range(NT):
        xtt, xtr = xt.next()
        slot = xt.i
        P.dma("sync", lambda e, t=t, xtt=xtt: e.dma_start(out=xtt[:], in_=xd[t * 128:(t + 1) * 128, :]), s_x[slot], writes=[xtr])
        c0t, c0r = c0.next()
        c1t, c1r = c1.next()
        ci = c0.i
        ro = (t % 8) * 128
        for rk in range(2):
            P.dma("gpsimd", lambda e, t=t, rk=rk, c0t=c0t, ro=ro: e.dma_start(
                out=c0t[:, rk * 512:(rk + 1) * 512], in_=o_g[t // 8, rk * 1024 + ro:rk * 1024 + ro + 128, :]), s_c0[ci][rk], writes=[c0r])
            P.dma("gpsimd", lambda e, t=t, rk=rk, c1t=c1t, ro=ro: e.dma_start(
                out=c1t[:, rk * 512:(rk + 1) * 512], in_=o_g[4 + t // 8, rk * 1024 + ro:rk * 1024 + ro + 128, :]), s_c1[ci][rk], writes=[c1r])
        oft, ofr = of.next()
        P.op("scalar", lambda e, oft=oft, c0t=c0t: e.activation(out=oft[:], in_=c0t[:], func=AF.Copy, scale=hcol_sb[:, 1:2]),
             reads=[c0r, r_hcol], writes=[ofr])
        P.op("vector", lambda e, oft=oft, c1t=c1t: e.scalar_tensor_tensor(out=oft[:], in0=c1t[:], scalar=hcol_sb[:, 0:1], in1=oft[:],
                                                                         op0=ALU.mult, op1=ALU.add),
             reads=[c1r, r_hcol, ofr], writes=[ofr])
        pd = (t % 2) * 2
        for k in range(8):
            b = pd + k // 4
            P.op("tensor", lambda e, k=k, b=b, oft=oft: e.transpose(out=PB[b][:, (k % 4) * 128:(k % 4 + 1) * 128],
                                                                   in_=oft[:, k * 128:(k + 1) * 128], identity=idf[:]),
                 reads=[ofr, r_id], writes=[RB[b]])
        oTt, oTr = oT.next()
        for hb in range(2):
            P.op("vector", lambda e, oTt=oTt, pd=pd, hb=hb: e.tensor_copy(out=oTt[:, hb * 4:(hb + 1) * 4, :],
                                                                        in_=PB[pd + hb][:].rearrange("p (k t) -> p k t", k=4)),
                 reads=[RB[pd + hb]], writes=[oTr])
        py = 4 + (t % 2) * 2
        for hf in range(2):
            for k in range(8):
                P.op("tensor", lambda e, k=k, hf=hf, py=py, oTt=oTt: e.matmul(
                    PB[py + hf][:], lhsT=oTt[:, k, :], rhs=wob[:, k, hf * 512:(hf + 1) * 512], start=(k == 0), stop=(k == 7)),
                    reads=[oTr, r_wob], writes=[RB[py + hf]])
            cs = slice(hf * 512, (hf + 1) * 512)
            P.op("vector", lambda e, hf=hf, cs=cs, py=py, xtt=xtt: e.tensor_tensor(out=xtt[:, cs], in0=PB[py + hf][:], in1=xtt[:, cs],
                                                                                 op=ALU.add),
                 reads=[RB[py + hf], xtr], writes=[xtr])
        P.dma("sync", lambda e, t=t, xtt=xtt: e.dma_start(out=xd[t * 128:(t + 1) * 128, :], in_=xtt[:]), s_o[slot], reads=[xtr])


def emit_F(C, xd, fg, out):
    P = C.P
    fgbc = C.A("fgbc", [128, D], F32); r_fg = Res()
    xt = C.ring("xt", 3, [128, D], F32)
    junk = C.A("junk", [128, D], BF16); r_junk = Res()
    st = C.ring("st", 2, [128, 4], F32)
    s_x = [C.dsem() for _ in range(3)]
    s_o = [C.dsem() for _ in range(3)]
    P.dma("sync", lambda e: e.dma_start(out=fgbc[:], in_=fg[0, :].partition_broadcast(128)), C.dsem(), writes=[r_fg])
    outs = []
    for t in range(NT):
        xtt, xtr = xt.next()
        slot = xt.i
        P.dma("sync", lambda e, t=t, xtt=xtt: e.dma_start(out=xtt[:], in_=xd[t * 128:(t + 1) * 128, :]), s_x[slot], writes=[xtr])
        stt, str_ = st.next()
        emit_rstd(P, xtt[:], xtr, junk, r_junk, stt[:, 0:1], stt[:, 1:2], stt[:, 2:3], str_)
        P.op("vector", lambda e, xtt=xtt, stt=stt: e.scalar_tensor_tensor(out=xtt[:], in0=xtt[:], scalar=stt[:, 2:3], in1=fgbc[:],
                                                                         op0=ALU.mult, op1=ALU.mult),
             reads=[xtr, str_, r_fg], writes=[xtr])
        o = P.dma("sync", lambda e, t=t, xtt=xtt: e.dma_start(out=out[t * 128:(t + 1) * 128, :], in_=xtt[:]), s_o[slot], reads=[xtr])
        outs.append(o)
    return outs[-3:]


def emit_E(C, layer, xd, modT_sb, r_mod, modrows, rw, rb, ewi, ebiT, ewo, ebo, idf, r_id, ne=NE, ntb=4):
    P, PB, RB = C.P, C.PB, C.RB
    A = C.A
    m0 = layer * 48
    g2bc = A("g2bc", [128, D], F32); r_g2 = Res()
    rw_sb = A("rw_sb", [128, 8, NE], F32); r_rw = Res()
    rb_sb = A("rb_sb", [1, NE], F32); r_rb = Res()
    ones_f = A("ones_f", [1, 128], F32); ones_b = A("ones_b", [1, 128], BF16); r_ones = Res()
    ebiT_sb = A("ebiT_sb", [128, NE * 16], F32); r_ebi = Res()
    acc = A("acc", [128, 8, D], F32); r_acc = [Res() for _ in range(8)]
    hT = A("hT", [128, 8, 1024], BF16); r_hT = [Res() for _ in range(8)]
    hTf = A("hTf", [128, 8, 128], F32); r_hTf = Res()
    xn = A("xn", [128, D], F32); r_xn = Res()
    junk = A("junk", [128, D], BF16); r_junk = Res()
    xr = C.ring("xr", 2, [128, D], F32)
    st = C.ring("st", 2, [128, 4], F32)
    lg = C.ring("lg", 2, [128, 96], F32)
    t8 = C.ring("t8", 2, [128, 16], F32)
    G = A("G", [128, 8, NE], F32); r_G = [Res() for _ in range(8)]
    w_in = C.ring("w_in", 2, [128, 8, 2 * D], BF16)
    w_out = C.ring("w_out", 2, [128, 8, D], BF16)
    bo = C.ring("bo", 2, [1, D], BF16)
    act = [A("act%d" % s, [128, 8, 512], BF16) for s in range(2)]
    r_act = [[Res() for _ in range(8)] for _ in range(2)]
    gt = C.ring("gt", 2, [128, 512], F32)
    sg = C.ring("sg", 2, [128, 512], F32)
    ut = C.ring("ut", 2, [128, 512], F32)
    s_x = [C.dsem() for _ in range(2)]
    s_win = [C.dsem() for _ in range(2)]
    s_wout = [C.dsem() for _ in range(2)]
    s_bo = [C.dsem() for _ in range(2)]
    s_o = [C.dsem() for _ in range(2)]
    r0 = layer * 6
    P.dma("sync", lambda e: e.dma_start(out=g2bc[:], in_=modrows[0, (r0 + 5) * D:(r0 + 6) * D].partition_broadcast(128)), C.dsem(), writes=[r_g2])
    P.dma("sync", lambda e: e.dma_start(out=rw_sb[:], in_=rw[layer].rearrange("(k p) n -> p k n", p=128)), C.dsem(), writes=[r_rw])
    P.dma("sync", lambda e: e.dma_start(out=rb_sb[:], in_=rb[layer:layer + 1, :]), C.dsem(), writes=[r_rb])
    P.dma("sync", lambda e: e.dma_start(out=ebiT_sb[:], in_=ebiT[layer]), C.dsem(), writes=[r_ebi])
    P.op("vector", lambda e: e.memset(ones_f[:], 1.0), writes=[r_ones])
    P.op("vector", lambda e: e.memset(ones_b[:], 1.0), writes=[r_ones])
    py_i = [0]
    py_banks = [0, 1, 6, 7]
    gu_i = [0]
    gu_banks = [(2, 3), (4, 5)]
    pending = []

    def load_w(ex):
        wit, wir = w_in.next()
        wot, wor = w_out.next()
        bot, bor = bo.next()
        si = w_in.i
        P.dma("gpsimd", lambda e: e.dma_start(out=wit[:], in_=ewi[layer, ex].rearrange("(k p) n -> p k n", p=128)),
              s_win[si], writes=[wir])
        P.dma("gpsimd", lambda e: e.dma_start(out=wot[:], in_=ewo[layer, ex].rearrange("(k p) n -> p k n", p=128)),
              s_wout[si], writes=[wor])
        P.dma("gpsimd", lambda e: e.dma_start(out=bot[:], in_=ebo[layer * NE + ex:layer * NE + ex + 1, :]), s_bo[si], writes=[bor])
        return wit, wir, wot, wor, bot, bor

    for tb in range(ntb):
        for j in range(8):
            row0 = (tb * 8 + j) * 128
            xt_, xtr = xr.next()
            P.dma("sync", lambda e, xt_=xt_, row0=row0: e.dma_start(out=xt_[:], in_=xd[row0:row0 + 128, :]), s_x[xr.i], writes=[xtr])
            stt, str_ = st.next()
            emit_rstd(P, xt_[:], xtr, junk, r_junk, stt[:, 0:1], stt[:, 1:2], stt[:, 2:3], str_)
            P.op("vector", lambda e, xt_=xt_, stt=stt: e.tensor_scalar(out=xn[:], in0=xt_[:], scalar1=stt[:, 2:3],
                                                                      scalar2=None, op0=ALU.mult),
                 reads=[xtr, str_], writes=[r_xn])
            for k in range(8):
                b = k // 4
                P.op("tensor", lambda e, k=k, b=b: e.transpose(out=PB[b][:, (k % 4) * 128:(k % 4 + 1) * 128],
                                                               in_=xn[:, k * 128:(k + 1) * 128], identity=idf[:]),
                     reads=[r_xn, r_id], writes=[RB[b]])
            for k in range(8):
                b = k // 4
                P.op("scalar", lambda e, k=k, b=b: e.activation(out=hTf[:, k, :], in_=PB[b][:, (k % 4) * 128:(k % 4 + 1) * 128],
                                                                func=AF.Identity, scale=modT_sb[:, m0 + 4 * 8 + k:m0 + 4 * 8 + k + 1],
                                                                bias=modT_sb[:, m0 + 3 * 8 + k:m0 + 3 * 8 + k + 1]),
                     reads=[RB[b], r_mod], writes=[r_hTf])
            P.op("vector", lambda e, j=j: e.tensor_copy(out=hT[:, :, j * 128:(j + 1) * 128], in_=hTf[:]),
                 reads=[r_hTf], writes=[r_hT[j]])
            for k in range(8):
                P.op("tensor", lambda e, k=k: e.matmul(PB[2][:, 0:NE], lhsT=hTf[:, k, :], rhs=rw_sb[:, k, :],
                                                       start=(k == 0), stop=False),
                     reads=[r_hTf, r_rw], writes=[RB[2]])
            P.op("tensor", lambda e: e.matmul(PB[2][:, 0:NE], lhsT=ones_f[0:1, :], rhs=rb_sb[0:1, :], start=False, stop=True),
                 reads=[r_ones, r_rb], writes=[RB[2]])
            lgt, lgr = lg.next()
            t8t, t8r = t8.next()
            P.op("vector", lambda e, lgt=lgt: e.tensor_copy(out=lgt[:, 0:NE], in_=PB[2][:, 0:NE]), reads=[RB[2]], writes=[lgr])
            P.op("vector", lambda e, lgt=lgt, t8t=t8t: e.max(out=t8t[:, 0:8], in_=lgt[:, 0:NE]), reads=[lgr], writes=[t8r])
            P.op("vector", lambda e, t8t=t8t: e.tensor_scalar(out=t8t[:, 8:9], in0=t8t[:, 0:1], scalar1=-1.0, scalar2=None,
                                                              op0=ALU.mult), reads=[t8r], writes=[t8r])
            P.op("vector", lambda e, lgt=lgt, t8t=t8t: e.tensor_scalar(out=lgt[:, 32:64], in0=lgt[:, 0:NE], scalar1=t8t[:, 3:4],
                                                                       scalar2=None, op0=ALU.is_ge),
                 reads=[lgr, t8r], writes=[lgr])
            P.op("scalar", lambda e, lgt=lgt, t8t=t8t: e.activation(out=lgt[:, 64:96], in_=lgt[:, 0:NE], func=AF.Exp,
                                                                    bias=t8t[:, 8:9], scale=1.0),
                 reads=[lgr, t8r], writes=[lgr])
            P.op("vector", lambda e, lgt=lgt, t8t=t8t: e.scalar_tensor_tensor(out=lgt[:, 32:64], in0=lgt[:, 64:96], scalar=1.0,
                                                                              in1=lgt[:, 32:64], op0=ALU.mult, op1=ALU.mult,
                                                                              accum_out=t8t[:, 9:10]),
                 reads=[lgr], writes=[lgr, t8r])
            P.op("vector", lambda e, t8t=t8t: e.reciprocal(out=t8t[:, 10:11], in_=t8t[:, 9:10]), reads=[t8r], writes=[t8r])
            P.op("vector", lambda e, lgt=lgt, t8t=t8t, j=j: e.tensor_scalar(out=G[:, j, :], in0=lgt[:, 32:64], scalar1=t8t[:, 10:11],
                                                                            scalar2=None, op0=ALU.mult),
                 reads=[lgr, t8r], writes=[r_G[j]])

        for ex in range(ne):
            if tb == 0 and ex == 0:
                pending.append(load_w(0))
            nxt = tb * ne + ex + 1
            if nxt < ntb * ne:
                pending.append(load_w(nxt % ne))
            wit, wir, wot, wor, bot, bor = pending.pop(0)
            for s in range(2):
                toks = slice(s * 512, (s + 1) * 512)
                hres = r_hT[s * 4:(s + 1) * 4]
                for fc in range(8):
                    bg, bu = gu_banks[gu_i[0] % 2]
                    gu_i[0] += 1
                    for k in range(8):
                        P.op("tensor", lambda e, k=k, fc=fc, bg=bg, wit=wit, toks=toks: e.matmul(
                            PB[bg][:], lhsT=wit[:, k, fc * 128:(fc + 1) * 128], rhs=hT[:, k, toks], start=(k == 0), stop=(k == 7)),
                            reads=[wir] + hres, writes=[RB[bg]])
                    for k in range(8):
                        P.op("tensor", lambda e, k=k, fc=fc, bu=bu, wit=wit, toks=toks: e.matmul(
                            PB[bu][:], lhsT=wit[:, k, D + fc * 128:D + (fc + 1) * 128], rhs=hT[:, k, toks], start=(k == 0), stop=(k == 7)),
                            reads=[wir] + hres, writes=[RB[bu]])
                    gtt, gtr = gt.next()
                    sgt, sgr = sg.next()
                    utt, utr = ut.next()
                    cg = ex * 16 + fc
                    cu = ex * 16 + 8 + fc
                    P.op("vector", lambda e, gtt=gtt, bg=bg, cg=cg: e.tensor_scalar(out=gtt[:], in0=PB[bg][:], scalar1=ebiT_sb[:, cg:cg + 1],
                                                                                   scalar2=7.0, op0=ALU.add, op1=ALU.min),
                         reads=[RB[bg], r_ebi], writes=[gtr])
                    P.op("scalar", lambda e, gtt=gtt, sgt=sgt: e.activation(out=sgt[:], in_=gtt[:], func=AF.Sigmoid, scale=1.702),
                         reads=[gtr], writes=[sgr])
                    P.op("scalar", lambda e, utt=utt, bu=bu, cu=cu: e.activation(out=utt[:], in_=PB[bu][:], func=AF.Identity,
                                                                                bias=ebiT_sb[:, cu:cu + 1], scale=1.0),
                         reads=[RB[bu], r_ebi], writes=[utr])
                    P.op("gpsimd", lambda e, utt=utt: e.tensor_scalar(out=utt[:], in0=utt[:], scalar1=7.0, scalar2=-7.0,
                                                                      op0=ALU.min, op1=ALU.max),
                         reads=[utr], writes=[utr])
                    P.op("gpsimd", lambda e, gtt=gtt, sgt=sgt: e.tensor_tensor(out=sgt[:], in0=gtt[:], in1=sgt[:], op=ALU.mult),
                         reads=[gtr, sgr], writes=[sgr])
                    P.op("vector", lambda e, utt=utt, sgt=sgt, fc=fc, s=s: e.scalar_tensor_tensor(out=act[s][:, fc, :], in0=utt[:], scalar=1.0,
                                                                                                 in1=sgt[:], op0=ALU.add, op1=ALU.mult),
                         reads=[utr, sgr], writes=[r_act[s][fc]])
            for s in range(2):
                for t4 in range(4):
                    j = s * 4 + t4
                    for hf in range(2):
                        pb = py_banks[py_i[0] % 4]
                        py_i[0] += 1
                        cols = slice(hf * 512, (hf + 1) * 512)
                        for fc in range(8):
                            P.op("tensor", lambda e, fc=fc, t4=t4, pb=pb, wot=wot, cols=cols, s=s: e.matmul(
                                PB[pb][:], lhsT=act[s][:, fc, t4 * 128:(t4 + 1) * 128], rhs=wot[:, fc, cols], start=(fc == 0), stop=False),
                                reads=[r_act[s][fc], wor], writes=[RB[pb]])
                        P.op("tensor", lambda e, pb=pb, bot=bot, cols=cols: e.matmul(
                            PB[pb][:], lhsT=ones_b[0:1, :], rhs=bot[0:1, cols], start=False, stop=True),
                            reads=[r_ones, bor], writes=[RB[pb]])
                        if ex == 0:
                            P.op("vector", lambda e, pb=pb, j=j, ex=ex, cols=cols: e.tensor_scalar(
                                out=acc[:, j, cols], in0=PB[pb][:], scalar1=G[:, j, ex:ex + 1], scalar2=None, op0=ALU.mult),
                                reads=[RB[pb], r_G[j]], writes=[r_acc[j]])
                        else:
                            P.op("vector", lambda e, pb=pb, j=j, ex=ex, cols=cols: e.scalar_tensor_tensor(
                                out=acc[:, j, cols], in0=PB[pb][:], scalar=G[:, j, ex:ex + 1], in1=acc[:, j, cols],
                                op0=ALU.mult, op1=ALU.add),
                                reads=[RB[pb], r_G[j], r_acc[j]], writes=[r_acc[j]])
        for j in range(8):
            row0 = (tb * 8 + j) * 128
            xt_, xtr = xr.next()
            P.dma("sync", lambda e, xt_=xt_, row0=row0: e.dma_start(out=xt_[:], in_=xd[row0:row0 + 128, :]), s_x[xr.i], writes=[xtr])
            P.op("gpsimd", lambda e, j=j: e.tensor_tensor(out=acc[:, j, :], in0=acc[:, j, :], in1=g2bc[:], op=ALU.mult),
                 reads=[r_acc[j], r_g2], writes=[r_acc[j]])
            P.op("vector", lambda e, j=j, xt_=xt_: e.tensor_tensor(out=xt_[:], in0=xt_[:], in1=acc[:, j, :], op=ALU.add),
                 reads=[r_acc[j], xtr], writes=[xtr])
            P.dma("sync", lambda e, xt_=xt_, row0=row0: e.dma_start(out=xd[row0:row0 + 128, :], in_=xt_[:]), s_o[xr.i], reads=[xtr])


def build_fused(layers=(0, 1, 2, 3), ne=NE, ntb=4, nhp=4, nqb=NQB, ne_in=NE):
    nc = bass.Bass("TRN2", target_bir_lowering=False)
    x = _din(nc, "x", [TOK, D])
    cT = _din(nc, "cT", [128, 8])
    adaw = _din(nc, "adaw", [4, D, 6 * D])
    adab = _din(nc, "adab", [1, 24 * D])
    ng = _din(nc, "ng", [1, 8 * D])
    pw = _din(nc, "pw", [2, 4, 256, 256])
    pscale = _din(nc, "pscale", [2, D])
    poolA = _din(nc, "poolA", [16, 128, 128])
    wq = _din(nc, "wq", [2, D, 512]); wk = _din(nc, "wk", [2, D, 512]); wv = _din(nc, "wv", [2, D, 512])
    wf = _din(nc, "wf", [2, D, 8]); bfr = _din(nc, "bf", [1, 16])
    wo = _din(nc, "wo", [2, D, D])
    rw = _din(nc, "rw", [4, D, NE]); rb = _din(nc, "rb", [4, NE])
    ewi = _din(nc, "ewi", [4, ne_in, D, 2 * D]); ebiT = _din(nc, "ebiT", [4, 128, NE * 16])
    ewo = _din(nc, "ewo", [4, ne_in, D, D]); ebo = _din(nc, "ebo", [4 * NE, D])
    fg = _din(nc, "fg", [1, D])
    ident = _din(nc, "ident", [128, 128]); tri = _din(nc, "tri", [128, 128]); sel = _din(nc, "sel", [128, 128])
    maskb = _din(nc, "maskb", [4, 128, 512]); hcol = _din(nc, "hcol", [128, 2])
    out = _dout(nc, "out", [TOK, D])
    xd = nc.dram_tensor("xd", [TOK, D], F32).ap()
    modrows = nc.dram_tensor("modrows", [1, 24 * D], F32).ap()
    tail_loc = [nc.dram_tensor("tail_loc%d" % i, [128, D], F32).ap() for i in range(2)]
    tail_g = [nc.dram_tensor("tail_g%d" % i, [256, D], F32).ap() for i in range(2)]
    hT_loc = [nc.dram_tensor("hT_loc%d" % i, [8, 128, TOK], BF16).ap() for i in range(2)]
    hT_g = [nc.dram_tensor("hT_g%d" % i, [8, 256, TOK], BF16).ap() for i in range(2)]
    o_loc = [nc.dram_tensor("o_loc%d" % i, [8, 1024, 512], BF16).ap() for i in range(2)]
    o_g = [nc.dram_tensor("o_g%d" % i, [8, 2048, 512], BF16).ap() for i in range(2)]

    C = Ctx(nc)
    P = C.P
    modT_sb = nc.alloc_sbuf_tensor("modT_sb", [128, 4 * 48], F32); r_modT = Res()
    idf = nc.alloc_sbuf_tensor("idf", [128, 128], F32); r_id = Res()
    hcol_sb = nc.alloc_sbuf_tensor("hcol_sb", [128, 2], F32); r_hcol = Res()
    C.finalize_globals()
    P.dma("sync", lambda e: e.dma_start(out=idf[:], in_=ident[:, :]), C.dsem(), writes=[r_id])
    P.dma("sync", lambda e: e.dma_start(out=hcol_sb[:], in_=hcol[:, :]), C.dsem(), writes=[r_hcol])

    emit_M(C, cT, adaw, adab, ng, modrows)
    C.phase()
    emit_modT(C, modrows, modT_sb, r_modT)
    first = True
    for i in layers:
        j = i // 2
        if i % 2 == 0:
            xsrc = x if first else xd
            emit_tail(C, xsrc, tail_loc[j], tail_g[j])
            C.phase()
            emit_Pm(C, i, xsrc, xd, tail_g[j][0:128, :], modrows, pw[j], pscale[j:j + 1, :], poolA)
            C.phase()
        else:
            emit_Aa(C, i, x if first else xd, hT_loc[j], modT_sb, r_modT, idf, r_id)
            C.phase()
            emit_gather8(C, hT_loc[j], hT_g[j])
            C.phase()
            emit_Ab(C, j, hT_g[j], wq, wk, wv, wf, bfr, tri, sel, maskb, o_loc[j], idf, r_id, nhp=nhp, nqb=nqb)
            C.phase()
            emit_gather8(C, o_loc[j], o_g[j])
            C.phase()
            emit_Ac(C, i, j, xd, o_g[j], wo, modrows, hcol_sb, r_hcol, idf, r_id)
            C.phase()
        first = False
        emit_E(C, i, xd, modT_sb, r_modT, modrows, rw, rb, ewi, ebiT, ewo, ebo, idf, r_id, ne=ne, ntb=ntb)
        C.phase()
    outs = emit_F(C, xd, fg, out)
    P.emit(final_waits=outs)
    P.close()
    return nc


def attn_consts():
    i = np.arange(128)
    tri = (i[:, None] <= i[None, :]).astype(np.float32)
    sel = np.zeros((128, 128), np.float32); sel[127, :] = 1.0
    kl = np.arange(128)[:, None]; ql = np.arange(512)[None, :]
    maskb = np.stack([np.where(ql >= r * 128 + kl, 0.0, -30000.0) for r in range(4)]).astype(np.float32)
    return dict(ident=np.eye(128, dtype=np.float32), tri=tri, sel=sel, maskb=maskb)


def pool_consts(half):
    i = np.arange(128)[:, None].astype(np.int64)
    o = np.arange(128)[None, :].astype(np.int64)
    out = np.zeros((4, 4, 128, 128), np.float32)
    for g, w in enumerate((2, 4, 8, 16)):
        band = ((o - i >= 0) & (o - i < w)).astype(np.float64)
        eye = (i == o).astype(np.float64)
        ac = band / w - eye
        ap = ((o + 128 - i) < w).astype(np.float64) / w
        cnt = np.minimum(o + 1, w).astype(np.float64)
        out[0, g] = ac
        out[1, g] = ap
        if half == 0:
            out[2, g] = band / cnt - eye
            out[3, g] = 0.0
        else:
            out[2, g] = ac
            out[3, g] = ap
    return out.reshape(16, 128, 128)


def make_in_maps(x, c, norm_mix_g, norm_ffn_g, ada_w, ada_b, pool_w, pool_scale, fox_w_in, fox_b_f, fox_w_o,
                 router_w, router_b, exp_w_in, exp_b_in, exp_w_out, exp_b_out, final_g):
    f32 = np.float32
    A = lambda a: np.ascontiguousarray(np.asarray(a, f32))
    x = A(x); c = A(c)
    ada_w = A(ada_w); exp_w_in = A(exp_w_in); exp_w_out = A(exp_w_out)
    fox_w_in = np.asarray(fox_w_in, f32); fox_b_f = np.asarray(fox_b_f, f32)
    adab = A(np.asarray(ada_b, f32).reshape(1, -1))
    ng = A(np.stack([np.stack([np.asarray(norm_mix_g, f32)[l], np.asarray(norm_ffn_g, f32)[l]]) for l in range(4)]).reshape(1, -1))
    ebiT = A(np.asarray(exp_b_in, f32).reshape(4, NE, 16, 128).transpose(0, 3, 1, 2).reshape(4, 128, NE * 16))
    ebo = A(np.asarray(exp_b_out, f32).reshape(4 * NE, D))
    shared = dict(adaw=ada_w, adab=adab, ng=ng, pw=A(pool_w), pscale=A(pool_scale), wo=A(fox_w_o), rw=A(router_w), rb=A(router_b),
                  ewi=exp_w_in, ebiT=ebiT, ewo=exp_w_out, ebo=ebo, fg=A(np.asarray(final_g, f32)[None, :]), **attn_consts())
    fox_par = []
    for p in range(2):
        fox_par.append(dict(
            wq=A(fox_w_in[:, :, 512 * p:512 * p + 512]), wk=A(fox_w_in[:, :, D + 512 * p:D + 512 * p + 512]),
            wv=A(fox_w_in[:, :, 2 * D + 512 * p:2 * D + 512 * p + 512]), wf=A(fox_w_in[:, :, 3 * D + 8 * p:3 * D + 8 * p + 8]),
            bf=A(fox_b_f[:, 8 * p:8 * p + 8].reshape(1, 16)), poolA=pool_consts(p),
            hcol=A(np.tile(np.array([[float(p), 1.0 - float(p)]], f32), (128, 1)))))
    ims = []
    for cc in range(NCORES):
        b, h = cc // 2, cc % 2
        m = dict(shared)
        m.update(fox_par[h])
        m["x"] = A(x[b, h * TOK:(h + 1) * TOK])
        m["cT"] = A(c[b].reshape(8, 128).T)
        ims.append(m)
    return ims


_NC = None


def kernel(**inputs):
    global _NC
    if _NC is None:
        _NC = build_fused()
    ims = make_in_maps(**inputs)
    res = run_bass_kernel_spmd(_NC, ims, core_ids=list(range(NCORES)))
    out = np.zeros((4, SEQ, D), np.float32)
    for cc in range(NCORES):
        b, h = cc // 2, cc % 2
        out[b, h * TOK:(h + 1) * TOK] = res.results[cc]["out"]
    return out
```

```python
import numpy as np
import ml_dtypes
import concourse.bass as bass
import concourse.mybir as mybir
from concourse.bass_utils import run_bass_kernel_spmd

F32 = mybir.dt.float32
BF16 = mybir.dt.bfloat16
AF = mybir.ActivationFunctionType
ALU = mybir.AluOpType

ENGS = ("sync", "scalar", "vector", "gpsimd", "tensor")
NCORES = 8
D = 1024
SEQ = 8192
TOK = 4096
NT = TOK // 128
NE = 32
EPS = 1e-6
NKT = SEQ // 128
NQB = SEQ // 512
PAIRS = [[0, 1], [2, 3], [4, 5], [6, 7]]


class Res:
    __slots__ = ("name", "last_write", "reads")

    def __init__(self, name=""):
        self.name = name
        self.last_write = None
        self.reads = {}


class Op:
    __slots__ = ("eng", "fn", "deps", "signal", "dma_sem", "tick", "is_dma")

    def __init__(self, eng, fn, is_dma=False, dma_sem=None):
        self.eng = eng
        self.fn = fn
        self.deps = []
        self.signal = False
        self.is_dma = is_dma
        self.dma_sem = dma_sem
        self.tick = None


class DmaSem:
    def __init__(self, prog, name, inc=16):
        self.sem = prog.new_sem(name)
        self.count = 0
        self.inc = inc
        self.last_op = None
        prog.all_dsems.append(self)


class Prog:
    def __init__(self, nc):
        self.nc = nc
        self.ops = {e: [] for e in ENGS}
        self._sem_ctx = []
        self.eng_sem = {}
        self.all_dsems = []
        self.scr = None

    def new_sem(self, name):
        ctx = self.nc.semaphore(name)
        s = ctx.__enter__()
        self._sem_ctx.append(ctx)
        return s

    def _add(self, eng, fn, reads, writes, is_dma=False, dma_sem=None, extra_deps=()):
        op = Op(eng, fn, is_dma, dma_sem)
        self.ops[eng].append(op)
        deps = list(extra_deps)
        for r in reads:
            if r.last_write is not None:
                deps.append(r.last_write)
        for w in writes:
            if w.last_write is not None:
                deps.append(w.last_write)
            deps.extend(w.reads.values())
        seen = set()
        for d in deps:
            if d is op or id(d) in seen:
                continue
            seen.add(id(d))
            if d.eng == eng and not d.is_dma and eng == "tensor":
                continue
            op.deps.append(d)
            d.signal = True
        for r in reads:
            r.reads[eng] = op
        for w in writes:
            w.last_write = op
            w.reads = {}
        return op

    def op(self, eng, fn, reads=(), writes=(), extra_deps=()):
        return self._add(eng, fn, reads, writes, extra_deps=extra_deps)

    def dma(self, eng, fn, dsem, reads=(), writes=(), extra_deps=()):
        op = self._add(eng, fn, reads, writes, is_dma=True, dma_sem=dsem, extra_deps=extra_deps)
        dsem.count += dsem.inc
        op.tick = dsem.count
        op.signal = True
        dsem.last_op = op
        return op

    def barrier(self, pe_out, pe_res):
        nc = self.nc
        if self.scr is None:
            self.scr = {e: nc.alloc_sbuf_tensor("bar_scr_" + e, [128, 16], F32) for e in ENGS}
            self.scrb = nc.alloc_sbuf_tensor("bar_scrb", [128, 16], BF16)
            self.bres = {e: Res("bar_" + e) for e in ENGS}
            self.bres2 = {e: Res("bar2_" + e) for e in ENGS}
            self.bsem = DmaSem(self, "bar_dma")
            self.bsem2 = DmaSem(self, "bar_dma2")
            self.op("vector", lambda e: e.memset(self.scrb[:], 0.0), writes=[self.bres["tensor"]])
            self.op("vector", lambda e: e.memset(self.scr["sync"][:], 0.0), writes=[self.bres["sync"]])
            self.op("vector", lambda e: e.memset(self.scr["scalar"][:], 0.0), writes=[self.bres["scalar"]])
        scr = self.scr

        def tiny(eng, stage):
            c0 = stage * 4
            if eng == "vector":
                return lambda e: e.memset(scr["vector"][0:1, c0:c0 + 1], 0.0)
            if eng == "gpsimd":
                return lambda e: e.memset(scr["gpsimd"][0:1, c0:c0 + 1], 0.0)
            if eng == "scalar":
                return lambda e: e.activation(out=scr["scalar"][0:1, c0:c0 + 1], in_=scr["scalar"][0:1, 8:9], func=AF.Copy)
            if eng == "tensor":
                return lambda e: e.matmul(pe_out[0:1, 0:1], lhsT=self.scrb[0:1, 0:1], rhs=self.scrb[0:1, 0:1], start=True, stop=True)
            return None

        bops = []
        for eng in ENGS:
            if eng == "sync":
                o = self.dma("sync", lambda e: e.dma_start(out=scr["sync"][0:1, 0:4], in_=scr["sync"][0:1, 8:12]), self.bsem,
                             writes=[self.bres[eng]])
            elif eng == "tensor":
                o = self.op(eng, tiny(eng, 0), reads=[self.bres[eng]], writes=[pe_res])
            else:
                o = self.op(eng, tiny(eng, 0), writes=[self.bres[eng]])
            o.signal = True
            bops.append(o)
        outstanding = [ds.last_op for ds in self.all_dsems if ds.last_op is not None]
        for eng in ENGS:
            deps = bops + outstanding
            if eng == "sync":
                self.dma("sync", lambda e: e.dma_start(out=scr["sync"][0:1, 4:8], in_=scr["sync"][0:1, 12:16]), self.bsem2,
                         writes=[self.bres2[eng]], extra_deps=deps)
            elif eng == "tensor":
                self.op(eng, tiny(eng, 1), writes=[pe_res], extra_deps=deps)
            else:
                self.op(eng, tiny(eng, 1), writes=[self.bres2[eng]], extra_deps=deps)

    def emit(self, final_waits=()):
        nc = self.nc
        for e in ENGS:
            self.eng_sem[e] = self.new_sem("eng_" + e)
        for e in ENGS:
            t = 0
            for op in self.ops[e]:
                if op.is_dma:
                    continue
                if op.signal:
                    t += 1
                    op.tick = t
        prog = self

        def chan(op):
            return op.dma_sem.sem if op.is_dma else prog.eng_sem[op.eng]

        def replay(engname, e, extra_final=()):
            waited = {}

            def do_wait(d):
                s = chan(d)
                k = id(s)
                if waited.get(k, 0) >= d.tick:
                    return
                e.wait_ge(s, d.tick)
                waited[k] = d.tick

            for op in prog.ops[engname]:
                for d in op.deps:
                    do_wait(d)
                ins = op.fn(e)
                if op.is_dma:
                    if op.dma_sem.inc == 16:
                        ins.then_inc(op.dma_sem.sem, 16)
                    else:
                        ins.then_inc(op.dma_sem.sem)
                elif op.signal:
                    ins.then_inc(prog.eng_sem[engname], 1)
            for d in extra_final:
                do_wait(d)

        with nc.Block() as block:
            @block.sync
            def _(e):
                replay("sync", e, final_waits)

            @block.scalar
            def _(e):
                replay("scalar", e)

            @block.vector
            def _(e):
                replay("vector", e)

            @block.gpsimd
            def _(e):
                replay("gpsimd", e)

            @block.tensor
            def _(e):
                replay("tensor", e)

    def close(self):
        for ctx in reversed(self._sem_ctx):
            ctx.__exit__(None, None, None)
        self._sem_ctx = []


class Ring:
    def __init__(self, C, name, n, shape, dtype):
        self.t = [C.A("%s%d" % (name, i), shape, dtype) for i in range(n)]
        self.r = [Res("%s%d" % (name, i)) for i in range(n)]
        self.n = n
        self.i = -1

    def next(self):
        self.i = (self.i + 1) % self.n
        return self.t[self.i], self.r[self.i]


_DTSIZE = {F32: 4, BF16: 2}


class Ctx:
    def __init__(self, nc):
        self.nc = nc
        self.P = Prog(nc)
        self.PB = [nc.alloc_psum_tensor("pb%d" % i, [128, 512], F32) for i in range(8)]
        self.RB = [Res("pb%d" % i) for i in range(8)]
        self.pool = []
        self.pool_i = 0
        self.cpool = []
        self.cpool_i = 0
        self.uid = 0
        self.arena_base = None
        self.arena_size = 0
        self.off = 0

    def finalize_globals(self):
        nc = self.nc
        size = (nc.sbuf_bytes_remaining - 2048) // 64 * 64
        arena = nc.alloc_sbuf_tensor("arena", [128, size // 4], F32)
        self.arena_base = nc.lookup_mloc(arena).addr
        self.arena_size = size

    def A(self, name, shape, dtype):
        n = _DTSIZE[dtype]
        for s in shape[1:]:
            n *= s
        n = (n + 63) // 64 * 64
        assert self.off + n <= self.arena_size, ("SBUF arena overflow", name, self.off, n, self.arena_size)
        self.uid += 1
        h = self.nc.alloc_sbuf_tensor_at("%s_u%d" % (name, self.uid), list(shape), dtype, offset=self.arena_base + self.off)
        self.off += n
        return h

    def ring(self, name, n, shape, dtype):
        return Ring(self, name, n, shape, dtype)

    def dsem(self):
        if self.pool_i == len(self.pool):
            self.pool.append(DmaSem(self.P, "ds%d" % len(self.pool)))
        s = self.pool[self.pool_i]
        self.pool_i += 1
        return s

    def csem(self):
        if self.cpool_i == len(self.cpool):
            self.cpool.append(DmaSem(self.P, "cs%d" % len(self.cpool), inc=1))
        s = self.cpool[self.cpool_i]
        self.cpool_i += 1
        return s

    def phase(self):
        self.P.barrier(self.PB[7], self.RB[7])
        self.off = 0
        self.pool_i = 0
        self.cpool_i = 0


def _din(nc, name, shape, dt=F32):
    return nc.dram_tensor(name, list(shape), dt, kind="ExternalInput").ap()


def _dout(nc, name, shape, dt=F32):
    return nc.dram_tensor(name, list(shape), dt, kind="ExternalOutput").ap()


def emit_rstd(P, xt, xr, junk, junkr, ss, sd, rstd, sr):
    P.op("scalar", lambda e: e.activation(out=junk[:], in_=xt, func=AF.Square, accum_out=ss[:]),
         reads=[xr], writes=[junkr, sr])
    P.op("scalar", lambda e: e.activation(out=sd[:], in_=ss[:], func=AF.Sqrt, scale=1.0 / D, bias=EPS),
         reads=[sr], writes=[sr])
    P.op("vector", lambda e: e.reciprocal(out=rstd[:], in_=sd[:]), reads=[sr], writes=[sr])


def emit_M(C, cT, adaw, adab, ng, modrows):
    P, PB, RB = C.P, C.PB, C.RB
    c_sb = C.A("c_sb", [128, 8], F32); r_c = Res()
    cond = C.A("cond", [128, 8], F32); r_cond = Res()
    adab_r = C.ring("adab_sb", 2, [1, 6 * D], F32)
    ng_r = C.ring("ng_sb", 2, [1, 2 * D], F32)
    mod_r = C.ring("mod_sb", 2, [1, 6 * D], F32)
    wch = C.ring("wch", 2, [128, 8, 512], F32)
    s_w = [C.dsem() for _ in range(2)]
    s_ab = [C.dsem() for _ in range(2)]
    s_ng = [C.dsem() for _ in range(2)]
    s_mo = [C.dsem() for _ in range(2)]
    P.dma("sync", lambda e: e.dma_start(out=c_sb[:], in_=cT[:, :]), C.dsem(), writes=[r_c])
    P.op("scalar", lambda e: e.activation(out=cond[:], in_=c_sb[:], func=AF.Silu), reads=[r_c], writes=[r_cond])
    i = 0
    for li in range(4):
        abt, abr = adab_r.next()
        ngt, ngr = ng_r.next()
        mt, mr = mod_r.next()
        sl = adab_r.i
        P.dma("sync", lambda e, abt=abt, li=li: e.dma_start(out=abt[:], in_=adab[0:1, li * 6 * D:(li + 1) * 6 * D]), s_ab[sl], writes=[abr])
        P.dma("sync", lambda e, ngt=ngt, li=li: e.dma_start(out=ngt[:], in_=ng[0:1, li * 2 * D:(li + 1) * 2 * D]), s_ng[sl], writes=[ngr])
        for nch in range(12):
            wt, wr = wch.next()
            P.dma("sync", lambda e, wt=wt, li=li, nch=nch: e.dma_start(
                out=wt[:], in_=adaw[li, :, nch * 512:(nch + 1) * 512].rearrange("(k p) n -> p k n", p=128)),
                s_w[wch.i], writes=[wr])
            b = i % 2
            i += 1
            for k in range(8):
                P.op("tensor", lambda e, k=k, b=b, wt=wt: e.matmul(PB[b][0:1, :], lhsT=cond[:, k:k + 1], rhs=wt[:, k, :],
                                                                  start=(k == 0), stop=(k == 7)),
                     reads=[r_cond, wr], writes=[RB[b]])
            c0 = nch * 512
            P.op("vector", lambda e, b=b, c0=c0, mt=mt, abt=abt: e.tensor_tensor(out=mt[0:1, c0:c0 + 512], in0=PB[b][0:1, :],
                                                                                in1=abt[0:1, c0:c0 + 512], op=ALU.add),
                 reads=[RB[b], abr], writes=[mr])
        for (v, gi) in ((1, 0), (4, 1)):
            c0 = v * D
            g0 = gi * D
            P.op("vector", lambda e, c0=c0, g0=g0, mt=mt, ngt=ngt: e.scalar_tensor_tensor(
                out=mt[0:1, c0:c0 + D], in0=mt[0:1, c0:c0 + D], scalar=1.0, in1=ngt[0:1, g0:g0 + D], op0=ALU.add, op1=ALU.mult),
                 reads=[mr, ngr], writes=[mr])
        P.dma("sync", lambda e, mt=mt, li=li: e.dma_start(out=modrows[0:1, li * 6 * D:(li + 1) * 6 * D], in_=mt[:]), s_mo[sl], reads=[mr])


def emit_modT(C, modrows, modT_sb, r_modT):
    P = C.P
    for lv in range(24):
        P.dma("sync", lambda e, lv=lv: e.dma_start(out=modT_sb[:, lv * 8:(lv + 1) * 8],
                                                   in_=modrows[0, lv * D:(lv + 1) * D].rearrange("(k p) -> p k", p=128),
                                                   allow_slow_non_contiguous=True),
              C.dsem(), writes=[r_modT])


def emit_tail(C, xsrc, tail_loc, tail_g):
    P = C.P
    r_t = Res()
    P.dma("gpsimd", lambda e: e.dma_start(out=tail_loc[:, :], in_=xsrc[TOK - 128:TOK, :]), C.dsem(), writes=[r_t])
    P.dma("gpsimd", lambda e: e.collective_compute("AllGather", ALU.bypass, replica_groups=PAIRS,
                                                   ins=[tail_loc[:, :]], outs=[tail_g[:, :]]), C.csem(), reads=[r_t])


def emit_Pm(C, layer, xsrc, xdst, xh, modrows, pw, pscale, poolA):
    P, PB, RB = C.P, C.PB, C.RB
    a1bc = C.A("a1bc", [128, D], F32); sh1bc = C.A("sh1bc", [128, D], F32); r_bc = Res()
    g1bc = C.A("g1bc", [128, D], F32); psbc = C.A("psbc", [128, D], F32); r_s = Res()
    pwf = C.A("pwf", [128, 8, 256], F32); r_pwf = Res()
    Wb = C.A("Wb", [128, 8, 256], BF16); r_Wb = Res()
    Af = C.A("Af", [128, 16, 128], F32); r_Af = Res()
    Abf = C.A("Abf", [128, 16, 128], BF16); r_Ab = Res()
    xt = C.ring("xt", 3, [128, D], F32)
    tmp = C.ring("tmp", 2, [128, D], F32)
    hr = C.ring("hr", 3, [128, D], BF16)
    junk = C.A("junk", [128, D], BF16); r_junk = Res()
    st = C.ring("st", 2, [128, 4], F32)
    dT = C.ring("dT", 2, [128, 8, 128], BF16)
    s_x = [C.dsem() for _ in range(3)]
    s_o = [C.dsem() for _ in range(3)]
    r0 = layer * 6
    P.dma("sync", lambda e: e.dma_start(out=sh1bc[:], in_=modrows[0, (r0 + 0) * D:(r0 + 1) * D].partition_broadcast(128)), C.dsem(), writes=[r_bc])
    P.dma("sync", lambda e: e.dma_start(out=a1bc[:], in_=modrows[0, (r0 + 1) * D:(r0 + 2) * D].partition_broadcast(128)), C.dsem(), writes=[r_bc])
    P.dma("sync", lambda e: e.dma_start(out=g1bc[:], in_=modrows[0, (r0 + 2) * D:(r0 + 3) * D].partition_broadcast(128)), C.dsem(), writes=[r_s])
    P.dma("sync", lambda e: e.dma_start(out=psbc[:], in_=pscale[0, :].partition_broadcast(128)), C.dsem(), writes=[r_s])
    P.dma("sync", lambda e: e.dma_start(out=pwf[:], in_=pw.rearrange("g (kk p) n -> p (g kk) n", p=128)), C.dsem(), writes=[r_pwf])
    P.dma("sync", lambda e: e.dma_start(out=Af[:], in_=poolA.rearrange("i p n -> p i n")), C.dsem(), writes=[r_Af])
    P.op("vector", lambda e: e.tensor_tensor(out=g1bc[:], in0=g1bc[:], in1=psbc[:], op=ALU.mult), reads=[r_s], writes=[r_s])
    for k in range(8):
        g = k // 2
        P.op("vector", lambda e, k=k, g=g: e.tensor_tensor(out=Wb[:, k, :], in0=pwf[:, k, :], in1=g1bc[:, g * 256:(g + 1) * 256],
                                                           op=ALU.mult), reads=[r_pwf, r_s], writes=[r_Wb])
    P.op("vector", lambda e: e.tensor_copy(out=Abf[:], in_=Af[:]), reads=[r_Af], writes=[r_Ab])

    def make_h(src_ap, xtt, xtr, slot):
        P.dma("sync", lambda e: e.dma_start(out=xtt[:], in_=src_ap), s_x[slot], writes=[xtr])
        stt, str_ = st.next()
        emit_rstd(P, xtt[:], xtr, junk, r_junk, stt[:, 0:1], stt[:, 1:2], stt[:, 2:3], str_)
        tt, tr = tmp.next()
        P.op("vector", lambda e: e.scalar_tensor_tensor(out=tt[:], in0=xtt[:], scalar=stt[:, 2:3], in1=a1bc[:],
                                                        op0=ALU.mult, op1=ALU.mult), reads=[xtr, str_, r_bc], writes=[tr])
        ht, hrr = hr.next()
        P.op("gpsimd", lambda e: e.tensor_tensor(out=ht[:], in0=tt[:], in1=sh1bc[:], op=ALU.add), reads=[tr, r_bc], writes=[hrr])
        return ht, hrr

    xtt, xtr = xt.next()
    hprev, hprev_r = make_h(xh, xtt, xtr, xt.i)
    for t in range(NT):
        xtt, xtr = xt.next()
        slot = xt.i
        hcur, hcur_r = make_h(xsrc[t * 128:(t + 1) * 128, :], xtt, xtr, slot)
        kc, kp = (2, 3) if t == 0 else (0, 1)
        pd = (t % 2) * 2
        for k in range(8):
            g = k // 2
            b = pd + k // 4
            cs = slice((k % 4) * 128, (k % 4 + 1) * 128)
            P.op("tensor", lambda e, k=k, b=b, cs=cs, g=g, hp=hprev, kp=kp: e.matmul(
                PB[b][:, cs], lhsT=hp[:, k * 128:(k + 1) * 128], rhs=Abf[:, kp * 4 + g, :], start=True, stop=False),
                reads=[hprev_r, r_Ab], writes=[RB[b]])
            P.op("tensor", lambda e, k=k, b=b, cs=cs, g=g, hc=hcur, kc=kc: e.matmul(
                PB[b][:, cs], lhsT=hc[:, k * 128:(k + 1) * 128], rhs=Abf[:, kc * 4 + g, :], start=False, stop=True),
                reads=[hcur_r, r_Ab], writes=[RB[b]])
        dt_, dr = dT.next()
        for hb in range(2):
            P.op("scalar", lambda e, hb=hb, pd=pd, dt_=dt_: e.activation(
                out=dt_[:, hb * 4:(hb + 1) * 4, :], in_=PB[pd + hb][:].rearrange("p (k t) -> p k t", k=4), func=AF.Copy),
                reads=[RB[pd + hb]], writes=[dr])
        py = 4 + (t % 2) * 2
        for g in range(4):
            b = py + g // 2
            cs = slice((g % 2) * 256, (g % 2 + 1) * 256)
            for kk in range(2):
                k = 2 * g + kk
                P.op("tensor", lambda e, k=k, b=b, cs=cs, kk=kk, dt_=dt_: e.matmul(
                    PB[b][:, cs], lhsT=dt_[:, k, :], rhs=Wb[:, k, :], start=(kk == 0), stop=(kk == 1)),
                    reads=[dr, r_Wb], writes=[RB[b]])
        for hb in range(2):
            cs = slice(hb * 512, (hb + 1) * 512)
            P.op("vector", lambda e, hb=hb, cs=cs, py=py, xtt=xtt: e.tensor_tensor(out=xtt[:, cs], in0=PB[py + hb][:], in1=xtt[:, cs],
                                                                                 op=ALU.add),
                 reads=[RB[py + hb], xtr], writes=[xtr])
        P.dma("sync", lambda e, t=t, xtt=xtt: e.dma_start(out=xdst[t * 128:(t + 1) * 128, :], in_=xtt[:]), s_o[slot], reads=[xtr])
        hprev, hprev_r = hcur, hcur_r


def emit_Aa(C, layer, xd, hT_loc, modT_sb, r_modT, idf, r_id):
    P, PB, RB = C.P, C.PB, C.RB
    xt = C.ring("xt", 3, [128, D], F32)
    xn = C.ring("xn", 2, [128, D], F32)
    junk = C.A("junk", [128, D], BF16); r_junk = Res()
    st = C.ring("st", 2, [128, 4], F32)
    ht = C.ring("ht", 3, [128, 8, 128], BF16)
    s_x = [C.dsem() for _ in range(3)]
    s_o = [C.dsem() for _ in range(3)]
    m0 = layer * 48
    hTv = hT_loc.rearrange("k p t -> p k t")
    for t in range(NT):
        xtt, xtr = xt.next()
        P.dma("sync", lambda e, t=t, xtt=xtt: e.dma_start(out=xtt[:], in_=xd[t * 128:(t + 1) * 128, :]), s_x[xt.i], writes=[xtr])
        stt, str_ = st.next()
        emit_rstd(P, xtt[:], xtr, junk, r_junk, stt[:, 0:1], stt[:, 1:2], stt[:, 2:3], str_)
        xnt, xnr = xn.next()
        P.op("vector", lambda e, xnt=xnt, xtt=xtt, stt=stt: e.tensor_scalar(out=xnt[:], in0=xtt[:], scalar1=stt[:, 2:3], scalar2=None,
                                                                          op0=ALU.mult), reads=[xtr, str_], writes=[xnr])
        pd = (t % 2) * 2
        for k in range(8):
            b = pd + k // 4
            P.op("tensor", lambda e, k=k, b=b, xnt=xnt: e.transpose(out=PB[b][:, (k % 4) * 128:(k % 4 + 1) * 128],
                                                                   in_=xnt[:, k * 128:(k + 1) * 128], identity=idf[:]),
                 reads=[xnr, r_id], writes=[RB[b]])
        htt, htr = ht.next()
        for k in range(8):
            b = pd + k // 4
            P.op("scalar", lambda e, k=k, b=b, htt=htt: e.activation(out=htt[:, k, :], in_=PB[b][:, (k % 4) * 128:(k % 4 + 1) * 128],
                                                                    func=AF.Identity, scale=modT_sb[:, m0 + 8 + k:m0 + 8 + k + 1],
                                                                    bias=modT_sb[:, m0 + k:m0 + k + 1]),
                 reads=[RB[b], r_modT], writes=[htr])
        P.dma("sync", lambda e, t=t, htt=htt: e.dma_start(out=hTv[:, :, t * 128:(t + 1) * 128], in_=htt[:]), s_o[ht.i], reads=[htr])


def emit_gather8(C, loc, gth):
    P = C.P
    for j in range(8):
        P.dma("gpsimd", lambda e, j=j: e.collective_compute("AllGather", ALU.bypass, replica_groups=PAIRS,
                                                            ins=[loc[j]], outs=[gth[j]]), C.csem())


def emit_Ab(C, j, hT_g, wq, wk, wv, wf, bfr, tri, sel, maskb, o_loc, idf, r_id, nhp=4, nqb=NQB):
    P, PB, RB = C.P, C.PB, C.RB
    wq_b = C.A("wq_b", [128, 8, 512], BF16); wk_b = C.A("wk_b", [128, 8, 512], BF16); wv_b = C.A("wv_b", [128, 8, 512], BF16)
    wf_b = C.A("wf_b", [128, 8, 8], BF16); r_w = Res()
    bf_sb = C.A("bf_sb", [128, 8], F32); nbf = C.A("nbf", [128, 8], F32); r_bf = Res()
    tri_sb = C.A("tri_sb", [128, 128], F32); sel_sb = C.A("sel_sb", [128, 128], F32)
    ones_sb = C.A("ones_sb", [128, 128], F32); r_cst = Res()
    mk = C.A("mk", [128, 4, 512], F32); r_mk = Res()
    qT = C.A("qT", [128, SEQ], BF16); r_qT = [Res() for _ in range(NQB)]
    kT = C.A("kT", [128, SEQ], BF16); r_kT = [Res() for _ in range(NQB)]
    V = C.A("V", [128, NKT, 2, 65], BF16); r_V = [Res() for _ in range(NQB)]
    NF = C.A("NF", [128, NKT, 2], F32); r_NF = [Res() for _ in range(NKT)]
    hblk = C.ring("hblk", 2, [128, 8, 512], BF16)
    e1 = C.ring("e1", 2, [128, 16], F32)
    dg = C.ring("dg", 2, [128, 128], F32)
    mnf = C.ring("mnf", 2, [128, 512], F32)
    comb = C.ring("comb", 2, [128, 4, 512], F32)
    tt = C.ring("tt", 6, [128, 512], F32)
    pT = C.ring("pT", 6, [128, 512], BF16)
    rec = C.ring("rec", 2, [128, 4], F32)
    ost = C.ring("ost", 2, [128, 4, 128], BF16)
    s_h = [C.dsem() for _ in range(2)]
    s_o = [C.dsem() for _ in range(2)]
    for (dst, src) in ((wq_b, wq[j]), (wk_b, wk[j]), (wv_b, wv[j]), (wf_b, wf[j])):
        P.dma("gpsimd", lambda e, dst=dst, src=src: e.dma_start(out=dst[:], in_=src.rearrange("(k p) n -> p k n", p=128)),
              C.dsem(), writes=[r_w])
    P.dma("sync", lambda e: e.dma_start(out=bf_sb[:], in_=bfr[0, j * 8:(j + 1) * 8].partition_broadcast(128)), C.dsem(), writes=[r_bf])
    for (dst, src) in ((tri_sb, tri), (sel_sb, sel)):
        P.dma("sync", lambda e, dst=dst, src=src: e.dma_start(out=dst[:], in_=src[:, :]), C.dsem(), writes=[r_cst])
    P.dma("sync", lambda e: e.dma_start(out=mk[:], in_=maskb.rearrange("r p n -> p r n")), C.dsem(), writes=[r_mk])
    P.op("vector", lambda e: e.memset(ones_sb[:], 1.0), writes=[r_cst])
    P.op("vector", lambda e: e.tensor_scalar(out=nbf[:], in0=bf_sb[:], scalar1=-1.0, scalar2=None, op0=ALU.mult),
         reads=[r_bf], writes=[r_bf])
    P.op("vector", lambda e: e.memset(V[:, :, :, 64:65], 1.0), writes=r_V)
    obi = [0]
    sring = [0]
    for hp in range(nhp):
        hs = slice(hp * 128, (hp + 1) * 128)
        for tb in range(NQB):
            ts_ = slice(tb * 512, (tb + 1) * 512)
            rk = tb // 8
            lo = (tb % 8) * 512
            hb, hbr = hblk.next()
            P.dma("sync", lambda e, hb=hb, rk=rk, lo=lo: e.dma_start(
                out=hb[:], in_=hT_g[:, rk * 128:(rk + 1) * 128, lo:lo + 512].rearrange("k p t -> p k t")), s_h[hblk.i], writes=[hbr])
            for k in range(8):
                P.op("tensor", lambda e, k=k, hb=hb, hs=hs: e.matmul(PB[0][:], lhsT=wq_b[:, k, hs], rhs=hb[:, k, :],
                                                                    start=(k == 0), stop=(k == 7)), reads=[r_w, hbr], writes=[RB[0]])
            P.op("scalar", lambda e, ts_=ts_: e.activation(out=qT[:, ts_], in_=PB[0][:], func=AF.Copy, scale=0.125),
                 reads=[RB[0]], writes=[r_qT[tb]])
            for k in range(8):
                P.op("tensor", lambda e, k=k, hb=hb, hs=hs: e.matmul(PB[1][:], lhsT=wk_b[:, k, hs], rhs=hb[:, k, :],
                                                                    start=(k == 0), stop=(k == 7)), reads=[r_w, hbr], writes=[RB[1]])
            P.op("vector", lambda e, ts_=ts_: e.tensor_copy(out=kT[:, ts_], in_=PB[1][:]), reads=[RB[1]], writes=[r_kT[tb]])
            for t4 in range(4):
                cs = slice(t4 * 128, (t4 + 1) * 128)
                for k in range(8):
                    P.op("tensor", lambda e, k=k, hb=hb, hs=hs, cs=cs: e.matmul(PB[2][:, cs], lhsT=hb[:, k, cs], rhs=wv_b[:, k, hs],
                                                                               start=(k == 0), stop=(k == 7)),
                         reads=[r_w, hbr], writes=[RB[2]])
            P.op("scalar", lambda e, tb=tb: e.activation(out=V[:, tb * 4:(tb + 1) * 4, :, 0:64],
                                                         in_=PB[2][:].rearrange("p (t h d) -> p t h d", t=4, h=2), func=AF.Copy),
                 reads=[RB[2]], writes=[r_V[tb]])
            for t4 in range(4):
                cs = slice(t4 * 128, (t4 + 1) * 128)
                for k in range(8):
                    P.op("tensor", lambda e, k=k, hb=hb, cs=cs, t4=t4, hp=hp: e.matmul(
                        PB[3][:, t4 * 2:(t4 + 1) * 2], lhsT=hb[:, k, cs], rhs=wf_b[:, k, hp * 2:(hp + 1) * 2], start=(k == 0), stop=(k == 7)),
                        reads=[r_w, hbr], writes=[RB[3]])
            e1t, e1r = e1.next()
            for h2 in range(2):
                P.op("scalar", lambda e, h2=h2, e1t=e1t, hp=hp: e.activation(
                    out=e1t[:, 0:8].rearrange("p (t h) -> p t h", h=2)[:, :, h2:h2 + 1],
                    in_=PB[3][:, 0:8].rearrange("p (t h) -> p t h", h=2)[:, :, h2:h2 + 1],
                    func=AF.Exp, scale=-1.0, bias=nbf[:, hp * 2 + h2:hp * 2 + h2 + 1]),
                    reads=[RB[3], r_bf], writes=[e1r])
            P.op("scalar", lambda e, e1t=e1t: e.activation(out=e1t[:, 8:16], in_=e1t[:, 0:8], func=AF.Ln, bias=1.0, scale=1.0),
                 reads=[e1r], writes=[e1r])
            for t4 in range(4):
                kt = tb * 4 + t4
                cc = slice(16 + t4 * 2, 16 + (t4 + 1) * 2)
                first = (kt == 0)
                P.op("tensor", lambda e, e1t=e1t, t4=t4, cc=cc, first=first: e.matmul(
                    PB[3][:, cc], lhsT=tri_sb[:], rhs=e1t[:, 8 + t4 * 2:8 + (t4 + 1) * 2], start=True, stop=first),
                    reads=[r_cst, e1r], writes=[RB[3]])
                if not first:
                    P.op("tensor", lambda e, kt=kt, cc=cc: e.matmul(PB[3][:, cc], lhsT=sel_sb[:], rhs=NF[:, kt - 1, :], start=False, stop=True),
                         reads=[r_cst, r_NF[kt - 1]], writes=[RB[3]])
                P.op("vector", lambda e, kt=kt, cc=cc: e.tensor_copy(out=NF[:, kt, :], in_=PB[3][:, cc]), reads=[RB[3]], writes=[r_NF[kt]])
        for qb in range(nqb):
            qs = slice(qb * 512, (qb + 1) * 512)
            ostt, ostr = ost.next()
            for h2 in range(2):
                rows = slice(h2 * 64, (h2 + 1) * 64)
                for t4 in range(4):
                    qt = qb * 4 + t4
                    dgt, dgr = dg.next()
                    P.op("vector", lambda e, dgt=dgt, qt=qt, h2=h2: e.tensor_scalar(out=dgt[:], in0=idf[:], scalar1=NF[:, qt, h2:h2 + 1],
                                                                                  scalar2=None, op0=ALU.mult),
                         reads=[r_id, r_NF[qt]], writes=[dgr])
                    P.op("tensor", lambda e, dgt=dgt, t4=t4: e.matmul(PB[3][:, t4 * 128:(t4 + 1) * 128], lhsT=ones_sb[:], rhs=dgt[:],
                                                                     start=True, stop=True), reads=[r_cst, dgr], writes=[RB[3]])
                mt, mr = mnf.next()
                P.op("scalar", lambda e, mt=mt: e.activation(out=mt[:], in_=PB[3][:], func=AF.Copy, scale=-1.0), reads=[RB[3]], writes=[mr])
                ct, cr = comb.next()
                for r in range(4):
                    P.op("gpsimd", lambda e, ct=ct, mt=mt, r=r: e.tensor_tensor(out=ct[:, r, :], in0=mt[:], in1=mk[:, r, :], op=ALU.add),
                         reads=[mr, r_mk], writes=[cr])
                ob = 4 + (obi[0] % 2)
                obi[0] += 1
                nkt = 4 * qb + 4
                sbank = {}

                def issue_S(kt):
                    sb = (0, 1, 2, 6, 7)[sring[0] % 5]
                    sring[0] += 1
                    sbank[kt] = sb
                    P.op("tensor", lambda e, sb=sb, kt=kt, rows=rows, qs=qs: e.matmul(
                        PB[sb][:], lhsT=kT[rows, kt * 128:(kt + 1) * 128], rhs=qT[rows, qs], start=True, stop=True),
                        reads=[r_kT[kt // 4], r_qT[qb]], writes=[RB[sb]])

                LA = 4
                for k0 in range(min(LA, nkt)):
                    issue_S(k0)
                for kt in range(nkt):
                    if kt + LA < nkt:
                        issue_S(kt + LA)
                    sb = sbank[kt]
                    r = kt - 4 * qb
                    ttt, ttr = tt.next()
                    if r >= 0:
                        P.op("vector", lambda e, ttt=ttt, sb=sb, ct=ct, r=r: e.tensor_tensor(out=ttt[:], in0=PB[sb][:], in1=ct[:, r, :], op=ALU.add),
                             reads=[RB[sb], cr], writes=[ttr])
                    else:
                        P.op("vector", lambda e, ttt=ttt, sb=sb, mt=mt: e.tensor_tensor(out=ttt[:], in0=PB[sb][:], in1=mt[:], op=ALU.add),
                             reads=[RB[sb], mr], writes=[ttr])
                    ptt, ptr = pT.next()
                    P.op("scalar", lambda e, ptt=ptt, ttt=ttt, kt=kt, h2=h2: e.activation(out=ptt[:], in_=ttt[:], func=AF.Exp,
                                                                                        bias=NF[:, kt, h2:h2 + 1], scale=1.0),
                         reads=[ttr, r_NF[kt]], writes=[ptr])
                    for c in range(4):
                        if r > c:
                            continue
                        P.op("tensor", lambda e, ptt=ptt, c=c, ob=ob, kt=kt, h2=h2, qb=qb: e.matmul(
                            PB[ob][:, c * 65:(c + 1) * 65], lhsT=ptt[:, c * 128:(c + 1) * 128], rhs=V[:, kt, h2, :],
                            start=(kt == 0 and c == 0), stop=(kt == 4 * qb + c), skip_group_check=True),
                            reads=[ptr, r_V[kt // 4]], writes=[RB[ob]])
                rt, rr = rec.next()
                P.op("vector", lambda e, rt=rt, ob=ob: e.reciprocal(
                    out=rt[:], in_=PB[ob][:, 0:260].rearrange("p (c d) -> p c d", d=65)[:, :, 64]), reads=[RB[ob]], writes=[rr])
                for c in range(4):
                    P.op("vector", lambda e, rt=rt, ob=ob, c=c, h2=h2, ostt=ostt: e.tensor_scalar(
                        out=ostt[:, c, h2 * 64:(h2 + 1) * 64], in0=PB[ob][:, c * 65:c * 65 + 64], scalar1=rt[:, c:c + 1], scalar2=None,
                        op0=ALU.mult), reads=[RB[ob], rr], writes=[ostr])
            P.dma("sync", lambda e, ostt=ostt, qb=qb, hs=hs: e.dma_start(
                out=o_loc[qb // 2, (qb % 2) * 512:(qb % 2 + 1) * 512, hs].rearrange("(c p) d -> p c d", p=128), in_=ostt[:]),
                s_o[ost.i], reads=[ostr])


def emit_Ac(C, layer, j, xd, o_g, wo, modrows, hcol_sb, r_hcol, idf, r_id):
    P, PB, RB = C.P, C.PB, C.RB
    g1bc = C.A("g1bc", [128, D], F32); r_g1 = Res()
    wof = C.A("wof", [128, 8, D], F32); r_wof = Res()
    wob = C.A("wob", [128, 8, D], BF16); r_wob = Res()
    xt = C.ring("xt", 3, [128, D], F32)
    c0 = C.ring("c0", 2, [128, D], BF16)
    c1 = C.ring("c1", 2, [128, D], BF16)
    of = C.ring("of", 2, [128, D], F32)
    oT = C.ring("oT", 2, [128, 8, 128], BF16)
    s_x = [C.dsem() for _ in range(3)]
    s_c0 = [[C.dsem() for _ in range(2)] for _ in range(2)]
    s_c1 = [[C.dsem() for _ in range(2)] for _ in range(2)]
    s_o = [C.dsem() for _ in range(3)]
    r0 = layer * 6
    P.dma("sync", lambda e: e.dma_start(out=g1bc[:], in_=modrows[0, (r0 + 2) * D:(r0 + 3) * D].partition_broadcast(128)), C.dsem(), writes=[r_g1])
    P.dma("sync", lambda e: e.dma_start(out=wof[:], in_=wo[j].rearrange("(k p) n -> p k n", p=128)), C.dsem(), writes=[r_wof])
    for k in range(8):
        P.op("vector", lambda e, k=k: e.tensor_tensor(out=wob[:, k, :], in0=wof[:, k, :], in1=g1bc[:], op=ALU.mult),
             reads=[r_wof, r_g1], writes=[r_wob])
    for t in range(NT):
        xtt, xtr = xt.next()
        slot = xt.i
        P.dma("sync", lambda e, t=t, xtt=xtt: e.dma_start(out=xtt[:], in_=xd[t * 128:(t + 1) * 128, :]), s_x[slot], writes=[xtr])
        c0t, c0r = c0.next()
        c1t, c1r = c1.next()
        ci = c0.i
        ro = (t % 8) * 128
        for rk in range(2):
            P.dma("gpsimd", lambda e, t=t, rk=rk, c0t=c0t, ro=ro: e.dma_start(
                out=c0t[:, rk * 512:(rk + 1) * 512], in_=o_g[t // 8, rk * 1024 + ro:rk * 1024 + ro + 128, :]), s_c0[ci][rk], writes=[c0r])
            P.dma("gpsimd", lambda e, t=t, rk=rk, c1t=c1t, ro=ro: e.dma_start(
                out=c1t[:, rk * 512:(rk + 1) * 512], in_=o_g[4 + t // 8, rk * 1024 + ro:rk * 1024 + ro + 128, :]), s_c1[ci][rk], writes=[c1r])
        oft, ofr = of.next()
        P.op("scalar", lambda e, oft=oft, c0t=c0t: e.activation(out=oft[:], in_=c0t[:], func=AF.Copy, scale=hcol_sb[:, 1:2]),
             reads=[c0r, r_hcol], writes=[ofr])
        P.op("vector", lambda e, oft=oft, c1t=c1t: e.scalar_tensor_tensor(out=oft[:], in0=c1t[:], scalar=hcol_sb[:, 0:1], in1=oft[:],
                                                                         op0=ALU.mult, op1=ALU.add),
             reads=[c1r, r_hcol, ofr], writes=[ofr])
        pd = (t % 2) * 2
        for k in range(8):
            b = pd + k // 4
            P.op("tensor", lambda e, k=k, b=b, oft=oft: e.transpose(out=PB[b][:, (k % 4) * 128:(k % 4 + 1) * 128],
                                                                   in_=oft[:, k * 128:(k + 1) * 128], identity=idf[:]),
                 reads=[ofr, r_id], writes=[RB[b]])
        oTt, oTr = oT.next()
        for hb in range(2):
            P.op("vector", lambda e, oTt=oTt, pd=pd, hb=hb: e.tensor_copy(out=oTt[:, hb * 4:(hb + 1) * 4, :],
                                                                        in_=PB[pd + hb][:].rearrange("p (k t) -> p k t", k=4)),
                 reads=[RB[pd + hb]], writes=[oTr])
        py = 4 + (t % 2) * 2
        for hf in range(2):
            for k in range(8):
                P.op("tensor", lambda e, k=k, hf=hf, py=py, oTt=oTt: e.matmul(
                    PB[py + hf][:], lhsT=oTt[:, k, :], rhs=wob[:, k, hf * 512:(hf + 1) * 512], start=(k == 0), stop=(k == 7)),
                    reads=[oTr, r_wob], writes=[RB[py + hf]])
            cs = slice(hf * 512, (hf + 1) * 512)
            P.op("vector", lambda e, hf=hf, cs=cs, py=py, xtt=xtt: e.tensor_tensor(out=xtt[:, cs], in0=PB[py + hf][:], in1=xtt[:, cs],
                                                                                 op=ALU.add),
                 reads=[RB[py + hf], xtr], writes=[xtr])
        P.dma("sync", lambda e, t=t, xtt=xtt: e.dma_start(out=xd[t * 128:(t + 1) * 128, :], in_=xtt[:]), s_o[slot], reads=[xtr])


def emit_F(C, xd, fg, out):
    P = C.P
    fgbc = C.A("fgbc", [128, D], F32); r_fg = Res()
    xt = C.ring("xt", 3, [128, D], F32)
    junk = C.A("junk", [128, D], BF16); r_junk = Res()
    st = C.ring("st", 2, [128, 4], F32)
    s_x = [C.dsem() for _ in range(3)]
    s_o = [C.dsem() for _ in range(3)]
    P.dma("sync", lambda e: e.dma_start(out=fgbc[:], in_=fg[0, :].partition_broadcast(128)), C.dsem(), writes=[r_fg])
    outs = []
    for t in range(NT):
        xtt, xtr = xt.next()
        slot = xt.i
        P.dma("sync", lambda e, t=t, xtt=xtt: e.dma_start(out=xtt[:], in_=xd[t * 128:(t + 1) * 128, :]), s_x[slot], writes=[xtr])
        stt, str_ = st.next()
        emit_rstd(P, xtt[:], xtr, junk, r_junk, stt[:, 0:1], stt[:, 1:2], stt[:, 2:3], str_)
        P.op("vector", lambda e, xtt=xtt, stt=stt: e.scalar_tensor_tensor(out=xtt[:], in0=xtt[:], scalar=stt[:, 2:3], in1=fgbc[:],
                                                                         op0=ALU.mult, op1=ALU.mult),
             reads=[xtr, str_, r_fg], writes=[xtr])
        o = P.dma("sync", lambda e, t=t, xtt=xtt: e.dma_start(out=out[t * 128:(t + 1) * 128, :], in_=xtt[:]), s_o[slot], reads=[xtr])
        outs.append(o)
    return outs[-3:]


def emit_E(C, layer, xd, modT_sb, r_mod, modrows, rw, rb, ewi, ebiT, ewo, ebo, idf, r_id, ne=NE, ntb=4):
    P, PB, RB = C.P, C.PB, C.RB
    A = C.A
    m0 = layer * 48
    g2bc = A("g2bc", [128, D], F32); r_g2 = Res()
    rw_sb = A("rw_sb", [128, 8, NE], F32); r_rw = Res()
    rb_sb = A("rb_sb", [1, NE], F32); r_rb = Res()
    ones_f = A("ones_f", [1, 128], F32); ones_b = A("ones_b", [1, 128], BF16); r_ones = Res()
    ebiT_sb = A("ebiT_sb", [128, NE * 16], F32); r_ebi = Res()
    x_blk = A("x_blk", [128, 8, D], F32); r_xb = [Res() for _ in range(8)]
    hT = A("hT", [128, 8, 1024], BF16); r_hT = [Res() for _ in range(8)]
    hTf = A("hTf", [128, 8, 128], F32); r_hTf = Res()
    xn = A("xn", [128, D], F32); r_xn = Res()
    junk = A("junk", [128, D], BF16); r_junk = Res()
    st = C.ring("st", 2, [128, 4], F32)
    lg = C.ring("lg", 2, [128, 96], F32)
    t8 = C.ring("t8", 2, [128, 16], F32)
    G = A("G", [128, 8, NE], F32); r_G = [Res() for _ in range(8)]
    w_in = C.ring("w_in", 2, [128, 8, 2 * D], BF16)
    w_out = C.ring("w_out", 2, [128, 8, D], BF16)
    bo = C.ring("bo", 2, [1, D], BF16)
    act = A("act", [128, 8, 512], BF16); r_act = [Res() for _ in range(8)]
    gt = C.ring("gt", 2, [128, 512], F32)
    sg = C.ring("sg", 2, [128, 512], F32)
    ut = C.ring("ut", 2, [128, 512], F32)
    yt = C.ring("yt", 2, [128, 512], F32)
    s_x = [C.dsem() for _ in range(8)]
    s_win = [C.dsem() for _ in range(2)]
    s_wout = [C.dsem() for _ in range(2)]
    s_bo = [C.dsem() for _ in range(2)]
    s_o = [C.dsem() for _ in range(8)]
    r0 = layer * 6
    P.dma("sync", lambda e: e.dma_start(out=g2bc[:], in_=modrows[0, (r0 + 5) * D:(r0 + 6) * D].partition_broadcast(128)), C.dsem(), writes=[r_g2])
    P.dma("sync", lambda e: e.dma_start(out=rw_sb[:], in_=rw[layer].rearrange("(k p) n -> p k n", p=128)), C.dsem(), writes=[r_rw])
    P.dma("sync", lambda e: e.dma_start(out=rb_sb[:], in_=rb[layer:layer + 1, :]), C.dsem(), writes=[r_rb])
    P.dma("sync", lambda e: e.dma_start(out=ebiT_sb[:], in_=ebiT[layer]), C.dsem(), writes=[r_ebi])
    P.op("vector", lambda e: e.memset(ones_f[:], 1.0), writes=[r_ones])
    P.op("vector", lambda e: e.memset(ones_b[:], 1.0), writes=[r_ones])
    py_i = [0]
    py_banks = [0, 1, 6, 7]
    gu_i = [0]
    gu_banks = [(2, 3), (4, 5)]
    pending = []

    def load_w(ex):
        wit, wir = w_in.next()
        wot, wor = w_out.next()
        bot, bor = bo.next()
        si = w_in.i
        P.dma("gpsimd", lambda e: e.dma_start(out=wit[:], in_=ewi[layer, ex].rearrange("(k p) n -> p k n", p=128)),
              s_win[si], writes=[wir])
        P.dma("gpsimd", lambda e: e.dma_start(out=wot[:], in_=ewo[layer, ex].rearrange("(k p) n -> p k n", p=128)),
              s_wout[si], writes=[wor])
        P.dma("gpsimd", lambda e: e.dma_start(out=bot[:], in_=ebo[layer * NE + ex:layer * NE + ex + 1, :]), s_bo[si], writes=[bor])
        return wit, wir, wot, wor, bot, bor

    for tb in range(ntb):
        for j in range(8):
            row0 = (tb * 8 + j) * 128
            P.dma("sync", lambda e, j=j, row0=row0: e.dma_start(out=x_blk[:, j, :], in_=xd[row0:row0 + 128, :]),
                  s_x[j], writes=[r_xb[j]])
            stt, str_ = st.next()
            emit_rstd(P, x_blk[:, j, :], r_xb[j], junk, r_junk, stt[:, 0:1], stt[:, 1:2], stt[:, 2:3], str_)
            P.op("vector", lambda e, j=j, stt=stt: e.tensor_scalar(out=xn[:], in0=x_blk[:, j, :], scalar1=stt[:, 2:3],
                                                                   scalar2=None, op0=ALU.mult),
                 reads=[r_xb[j], str_], writes=[r_xn])
            for k in range(8):
                b = k // 4
                P.op("tensor", lambda e, k=k, b=b: e.transpose(out=PB[b][:, (k % 4) * 128:(k % 4 + 1) * 128],
                                                               in_=xn[:, k * 128:(k + 1) * 128], identity=idf[:]),
                     reads=[r_xn, r_id], writes=[RB[b]])
            for k in range(8):
                b = k // 4
                P.op("scalar", lambda e, k=k, b=b: e.activation(out=hTf[:, k, :], in_=PB[b][:, (k % 4) * 128:(k % 4 + 1) * 128],
                                                                func=AF.Identity, scale=modT_sb[:, m0 + 4 * 8 + k:m0 + 4 * 8 + k + 1],
                                                                bias=modT_sb[:, m0 + 3 * 8 + k:m0 + 3 * 8 + k + 1]),
                     reads=[RB[b], r_mod], writes=[r_hTf])
            P.op("vector", lambda e, j=j: e.tensor_copy(out=hT[:, :, j * 128:(j + 1) * 128], in_=hTf[:]),
                 reads=[r_hTf], writes=[r_hT[j]])
            for k in range(8):
                P.op("tensor", lambda e, k=k: e.matmul(PB[2][:, 0:NE], lhsT=hTf[:, k, :], rhs=rw_sb[:, k, :],
                                                       start=(k == 0), stop=False),
                     reads=[r_hTf, r_rw], writes=[RB[2]])
            P.op("tensor", lambda e: e.matmul(PB[2][:, 0:NE], lhsT=ones_f[0:1, :], rhs=rb_sb[0:1, :], start=False, stop=True),
                 reads=[r_ones, r_rb], writes=[RB[2]])
            lgt, lgr = lg.next()
            t8t, t8r = t8.next()
            P.op("vector", lambda e, lgt=lgt: e.tensor_copy(out=lgt[:, 0:NE], in_=PB[2][:, 0:NE]), reads=[RB[2]], writes=[lgr])
            P.op("vector", lambda e, lgt=lgt, t8t=t8t: e.max(out=t8t[:, 0:8], in_=lgt[:, 0:NE]), reads=[lgr], writes=[t8r])
            P.op("vector", lambda e, t8t=t8t: e.tensor_scalar(out=t8t[:, 8:9], in0=t8t[:, 0:1], scalar1=-1.0, scalar2=None,
                                                              op0=ALU.mult), reads=[t8r], writes=[t8r])
            P.op("vector", lambda e, lgt=lgt, t8t=t8t: e.tensor_scalar(out=lgt[:, 32:64], in0=lgt[:, 0:NE], scalar1=t8t[:, 3:4],
                                                                       scalar2=None, op0=ALU.is_ge),
                 reads=[lgr, t8r], writes=[lgr])
            P.op("scalar", lambda e, lgt=lgt, t8t=t8t: e.activation(out=lgt[:, 64:96], in_=lgt[:, 0:NE], func=AF.Exp,
                                                                    bias=t8t[:, 8:9], scale=1.0),
                 reads=[lgr, t8r], writes=[lgr])
            P.op("vector", lambda e, lgt=lgt, t8t=t8t: e.scalar_tensor_tensor(out=lgt[:, 32:64], in0=lgt[:, 64:96], scalar=1.0,
                                                                              in1=lgt[:, 32:64], op0=ALU.mult, op1=ALU.mult,
                                                                              accum_out=t8t[:, 9:10]),
                 reads=[lgr], writes=[lgr, t8r])
            P.op("vector", lambda e, t8t=t8t: e.reciprocal(out=t8t[:, 10:11], in_=t8t[:, 9:10]), reads=[t8r], writes=[t8r])
            P.op("vector", lambda e, lgt=lgt, t8t=t8t, j=j: e.tensor_scalar(out=G[:, j, :], in0=lgt[:, 32:64], scalar1=t8t[:, 10:11],
                                                                            scalar2=None, op0=ALU.mult),
                 reads=[lgr, t8r], writes=[r_G[j]])
        for ex in range(ne):
            if tb == 0 and ex == 0:
                pending.append(load_w(0))
            nxt = tb * ne + ex + 1
            if nxt < ntb * ne:
                pending.append(load_w(nxt % ne))
            wit, wir, wot, wor, bot, bor = pending.pop(0)
            for s in range(2):
                toks = slice(s * 512, (s + 1) * 512)
                hres = r_hT[s * 4:(s + 1) * 4]
                for fc in range(8):
                    bg, bu = gu_banks[gu_i[0] % 2]
                    gu_i[0] += 1
                    for k in range(8):
                        P.op("tensor", lambda e, k=k, fc=fc, bg=bg, wit=wit, toks=toks: e.matmul(
                            PB[bg][:], lhsT=wit[:, k, fc * 128:(fc + 1) * 128], rhs=hT[:, k, toks], start=(k == 0), stop=(k == 7)),
                            reads=[wir] + hres, writes=[RB[bg]])
                    for k in range(8):
                        P.op("tensor", lambda e, k=k, fc=fc, bu=bu, wit=wit, toks=toks: e.matmul(
                            PB[bu][:], lhsT=wit[:, k, D + fc * 128:D + (fc + 1) * 128], rhs=hT[:, k, toks], start=(k == 0), stop=(k == 7)),
                            reads=[wir] + hres, writes=[RB[bu]])
                    gtt, gtr = gt.next()
                    sgt, sgr = sg.next()
                    utt, utr = ut.next()
                    cg = ex * 16 + fc
                    cu = ex * 16 + 8 + fc
                    P.op("vector", lambda e, gtt=gtt, bg=bg, cg=cg: e.tensor_scalar(out=gtt[:], in0=PB[bg][:], scalar1=ebiT_sb[:, cg:cg + 1],
                                                                                   scalar2=7.0, op0=ALU.add, op1=ALU.min),
                         reads=[RB[bg], r_ebi], writes=[gtr])
                    P.op("scalar", lambda e, gtt=gtt, sgt=sgt: e.activation(out=sgt[:], in_=gtt[:], func=AF.Sigmoid, scale=1.702),
                         reads=[gtr], writes=[sgr])
                    P.op("scalar", lambda e, utt=utt, bu=bu, cu=cu: e.activation(out=utt[:], in_=PB[bu][:], func=AF.Identity,
                                                                                bias=ebiT_sb[:, cu:cu + 1], scale=1.0),
                         reads=[RB[bu], r_ebi], writes=[utr])
                    P.op("vector", lambda e, utt=utt: e.tensor_scalar(out=utt[:], in0=utt[:], scalar1=7.0, scalar2=-7.0,
                                                                      op0=ALU.min, op1=ALU.max),
                         reads=[utr], writes=[utr])
                    P.op("vector", lambda e, gtt=gtt, sgt=sgt: e.tensor_tensor(out=sgt[:], in0=gtt[:], in1=sgt[:], op=ALU.mult),
                         reads=[gtr, sgr], writes=[sgr])
                    P.op("vector", lambda e, utt=utt, sgt=sgt, fc=fc: e.scalar_tensor_tensor(out=act[:, fc, :], in0=utt[:], scalar=1.0,
                                                                                            in1=sgt[:], op0=ALU.add, op1=ALU.mult),
                         reads=[utr, sgr], writes=[r_act[fc]])
                for t4 in range(4):
                    j = s * 4 + t4
                    for hf in range(2):
                        pb = py_banks[py_i[0] % 4]
                        py_i[0] += 1
                        cols = slice(hf * 512, (hf + 1) * 512)
                        for fc in range(8):
                            P.op("tensor", lambda e, fc=fc, t4=t4, pb=pb, wot=wot, cols=cols: e.matmul(
                                PB[pb][:], lhsT=act[:, fc, t4 * 128:(t4 + 1) * 128], rhs=wot[:, fc, cols], start=(fc == 0), stop=False),
                                reads=[r_act[fc], wor], writes=[RB[pb]])
                        P.op("tensor", lambda e, pb=pb, bot=bot, cols=cols: e.matmul(
                            PB[pb][:], lhsT=ones_b[0:1, :], rhs=bot[0:1, cols], start=False, stop=True),
                            reads=[r_ones, bor], writes=[RB[pb]])
                        ytt, ytr = yt.next()
                        P.op("vector", lambda e, ytt=ytt, pb=pb, cols=cols: e.tensor_tensor(out=ytt[:], in0=PB[pb][:], in1=g2bc[:, cols],
                                                                                          op=ALU.mult),
                             reads=[RB[pb], r_g2], writes=[ytr])
                        P.op("vector", lambda e, ytt=ytt, j=j, ex=ex, cols=cols: e.scalar_tensor_tensor(
                            out=x_blk[:, j, cols], in0=ytt[:], scalar=G[:, j, ex:ex + 1], in1=x_blk[:, j, cols],
                            op0=ALU.mult, op1=ALU.add),
                            reads=[ytr, r_G[j], r_xb[j]], writes=[r_xb[j]])
        for j in range(8):
            row0 = (tb * 8 + j) * 128
            P.dma("sync", lambda e, j=j, row0=row0: e.dma_start(out=xd[row0:row0 + 128, :], in_=x_blk[:, j, :]),
                  s_o[j], reads=[r_xb[j]])


def build_fused(layers=(0, 1, 2, 3), ne=NE, ntb=4, nhp=4, nqb=NQB, ne_in=NE):
    nc = bass.Bass("TRN2", target_bir_lowering=False)
    x = _din(nc, "x", [TOK, D])
    cT = _din(nc, "cT", [128, 8])
    adaw = _din(nc, "adaw", [4, D, 6 * D])
    adab = _din(nc, "adab", [1, 24 * D])
    ng = _din(nc, "ng", [1, 8 * D])
    pw = _din(nc, "pw", [2, 4, 256, 256])
    pscale = _din(nc, "pscale", [2, D])
    poolA = _din(nc, "poolA", [16, 128, 128])
    wq = _din(nc, "wq", [2, D, 512]); wk = _din(nc, "wk", [2, D, 512]); wv = _din(nc, "wv", [2, D, 512])
    wf = _din(nc, "wf", [2, D, 8]); bfr = _din(nc, "bf", [1, 16])
    wo = _din(nc, "wo", [2, D, D])
    rw = _din(nc, "rw", [4, D, NE]); rb = _din(nc, "rb", [4, NE])
    ewi = _din(nc, "ewi", [4, ne_in, D, 2 * D]); ebiT = _din(nc, "ebiT", [4, 128, NE * 16])
    ewo = _din(nc, "ewo", [4, ne_in, D, D]); ebo = _din(nc, "ebo", [4 * NE, D])
    fg = _din(nc, "fg", [1, D])
    ident = _din(nc, "ident", [128, 128]); tri = _din(nc, "tri", [128, 128]); sel = _din(nc, "sel", [128, 128])
    maskb = _din(nc, "maskb", [4, 128, 512]); hcol = _din(nc, "hcol", [128, 2])
    out = _dout(nc, "out", [TOK, D])
    xd = nc.dram_tensor("xd", [TOK, D], F32).ap()
    modrows = nc.dram_tensor("modrows", [1, 24 * D], F32).ap()
    tail_loc = [nc.dram_tensor("tail_loc%d" % i, [128, D], F32).ap() for i in range(2)]
    tail_g = [nc.dram_tensor("tail_g%d" % i, [256, D], F32).ap() for i in range(2)]
    hT_loc = [nc.dram_tensor("hT_loc%d" % i, [8, 128, TOK], BF16).ap() for i in range(2)]
    hT_g = [nc.dram_tensor("hT_g%d" % i, [8, 256, TOK], BF16).ap() for i in range(2)]
    o_loc = [nc.dram_tensor("o_loc%d" % i, [8, 1024, 512], BF16).ap() for i in range(2)]
    o_g = [nc.dram_tensor("o_g%d" % i, [8, 2048, 512], BF16).ap() for i in range(2)]

    C = Ctx(nc)
    P = C.P
    modT_sb = nc.alloc_sbuf_tensor("modT_sb", [128, 4 * 48], F32); r_modT = Res()
    idf = nc.alloc_sbuf_tensor("idf", [128, 128], F32); r_id = Res()
    hcol_sb = nc.alloc_sbuf_tensor("hcol_sb", [128, 2], F32); r_hcol = Res()
    C.finalize_globals()
    P.dma("sync", lambda e: e.dma_start(out=idf[:], in_=ident[:, :]), C.dsem(), writes=[r_id])
    P.dma("sync", lambda e: e.dma_start(out=hcol_sb[:], in_=hcol[:, :]), C.dsem(), writes=[r_hcol])

    emit_M(C, cT, adaw, adab, ng, modrows)
    C.phase()
    emit_modT(C, modrows, modT_sb, r_modT)
    first = True
    for i in layers:
        j = i // 2
        if i % 2 == 0:
            xsrc = x if first else xd
            emit_tail(C, xsrc, tail_loc[j], tail_g[j])
            C.phase()
            emit_Pm(C, i, xsrc, xd, tail_g[j][0:128, :], modrows, pw[j], pscale[j:j + 1, :], poolA)
            C.phase()
        else:
            emit_Aa(C, i, x if first else xd, hT_loc[j], modT_sb, r_modT, idf, r_id)
            C.phase()
            emit_gather8(C, hT_loc[j], hT_g[j])
            C.phase()
            emit_Ab(C, j, hT_g[j], wq, wk, wv, wf, bfr, tri, sel, maskb, o_loc[j], idf, r_id, nhp=nhp, nqb=nqb)
            C.phase()
            emit_gather8(C, o_loc[j], o_g[j])
            C.phase()
            emit_Ac(C, i, j, xd, o_g[j], wo, modrows, hcol_sb, r_hcol, idf, r_id)
            C.phase()
        first = False
        emit_E(C, i, xd, modT_sb, r_modT, modrows, rw, rb, ewi, ebiT, ewo, ebo, idf, r_id, ne=ne, ntb=ntb)
        C.phase()
    outs = emit_F(C, xd, fg, out)
    P.emit(final_waits=outs)
    P.close()
    return nc


def attn_consts():
    i = np.arange(128)
    tri = (i[:, None] <= i[None, :]).astype(np.float32)
    sel = np.zeros((128, 128), np.float32); sel[127, :] = 1.0
    kl = np.arange(128)[:, None]; ql = np.arange(512)[None, :]
    maskb = np.stack([np.where(ql >= r * 128 + kl, 0.0, -30000.0) for r in range(4)]).astype(np.float32)
    return dict(ident=np.eye(128, dtype=np.float32), tri=tri, sel=sel, maskb=maskb)


def pool_consts(half):
    i = np.arange(128)[:, None].astype(np.int64)
    o = np.arange(128)[None, :].astype(np.int64)
    out = np.zeros((4, 4, 128, 128), np.float32)
    for g, w in enumerate((2, 4, 8, 16)):
        band = ((o - i >= 0) & (o - i < w)).astype(np.float64)
        eye = (i == o).astype(np.float64)
        ac = band / w - eye
        ap = ((o + 128 - i) < w).astype(np.float64) / w
        cnt = np.minimum(o + 1, w).astype(np.float64)
        out[0, g] = ac
        out[1, g] = ap
        if half == 0:
            out[2, g] = band / cnt - eye
            out[3, g] = 0.0
        else:
            out[2, g] = ac
            out[3, g] = ap
    return out.reshape(16, 128, 128)


def make_in_maps(x, c, norm_mix_g, norm_ffn_g, ada_w, ada_b, pool_w, pool_scale, fox_w_in, fox_b_f, fox_w_o,
                 router_w, router_b, exp_w_in, exp_b_in, exp_w_out, exp_b_out, final_g):
    f32 = np.float32
    A = lambda a: np.ascontiguousarray(np.asarray(a, f32))
    x = A(x); c = A(c)
    ada_w = A(ada_w); exp_w_in = A(exp_w_in); exp_w_out = A(exp_w_out)
    fox_w_in = np.asarray(fox_w_in, f32); fox_b_f = np.asarray(fox_b_f, f32)
    adab = A(np.asarray(ada_b, f32).reshape(1, -1))
    ng = A(np.stack([np.stack([np.asarray(norm_mix_g, f32)[l], np.asarray(norm_ffn_g, f32)[l]]) for l in range(4)]).reshape(1, -1))
    ebiT = A(np.asarray(exp_b_in, f32).reshape(4, NE, 16, 128).transpose(0, 3, 1, 2).reshape(4, 128, NE * 16))
    ebo = A(np.asarray(exp_b_out, f32).reshape(4 * NE, D))
    shared = dict(adaw=ada_w, adab=adab, ng=ng, pw=A(pool_w), pscale=A(pool_scale), wo=A(fox_w_o), rw=A(router_w), rb=A(router_b),
                  ewi=exp_w_in, ebiT=ebiT, ewo=exp_w_out, ebo=ebo, fg=A(np.asarray(final_g, f32)[None, :]), **attn_consts())
    fox_par = []
    for p in range(2):
        fox_par.append(dict(
            wq=A(fox_w_in[:, :, 512 * p:512 * p + 512]), wk=A(fox_w_in[:, :, D + 512 * p:D + 512 * p + 512]),
            wv=A(fox_w_in[:, :, 2 * D + 512 * p:2 * D + 512 * p + 512]), wf=A(fox_w_in[:, :, 3 * D + 8 * p:3 * D + 8 * p + 8]),
            bf=A(fox_b_f[:, 8 * p:8 * p + 8].reshape(1, 16)), poolA=pool_consts(p),
            hcol=A(np.tile(np.array([[float(p), 1.0 - float(p)]], f32), (128, 1)))))
    ims = []
    for cc in range(NCORES):
        b, h = cc // 2, cc % 2
        m = dict(shared)
        m.update(fox_par[h])
        m["x"] = A(x[b, h * TOK:(h + 1) * TOK])
        m["cT"] = A(c[b].reshape(8, 128).T)
        ims.append(m)
    return ims


_NC = None


def kernel(**inputs):
    global _NC
    if _NC is None:
        _NC = build_fused()
    ims = make_in_maps(**inputs)
    res = run_bass_kernel_spmd(_NC, ims, core_ids=list(range(NCORES)))
    out = np.zeros((4, SEQ, D), np.float32)
    for cc in range(NCORES):
        b, h = cc // 2, cc % 2
        out[b, h * TOK:(h + 1) * TOK] = res.results[cc]["out"]
    return out
```
